# Optimizing a Trainium2 kernel written in Bass

```python
import math
import numpy as np
import jax
import jax.numpy as jnp
from jax import lax

D_MODEL = 2048
BATCH = 8
SEQ = 4096
DEPTH = 4

CTX_LEN = 256
GRID_W = 64
MIX_W = D_MODEL // 2
N_EVEN = (DEPTH + 1) // 2
N_ODD = DEPTH // 2
N_MOD = 6
NORM_EPS = 1e-6

S5_GROUP = 16
S5_GROUPS = MIX_W // S5_GROUP
S5_STATE = 64
S5_DT_MIN = 1e-3
S5_DT_MAX = 1e-1

NA_HEADS = 8
NA_HEAD_DIM = MIX_W // NA_HEADS
WIN_ROWS = 8
WIN_COLS = 16

RK_HEAD = 64
RK_HEADS = MIX_W // RK_HEAD
RK_DECAY_RANK = 64
RK_ICLR_RANK = 64
RK_GATE_RANK = 128
RK_GN_EPS = 64e-5
RK_SPLITS = (MIX_W, MIX_W, MIX_W, RK_GATE_RANK, 2 * RK_DECAY_RANK, 2 * RK_ICLR_RANK)
RK_IN = sum(RK_SPLITS)

GQ_HEAD_DIM = 128
GQ_HEADS = MIX_W // GQ_HEAD_DIM
GQ_KV_HEADS = GQ_HEADS // 4
KV_W = GQ_KV_HEADS * GQ_HEAD_DIM
ROPE_THETA = 10000.0
Q_BLOCK = 128

EVEN_SPLITS = (MIX_W, MIX_W, MIX_W, MIX_W)
EVEN_IN = sum(EVEN_SPLITS)
ODD_SPLITS = (RK_IN, MIX_W, KV_W, KV_W)
ODD_IN = sum(ODD_SPLITS)

N_EXPERTS = 32
TOP_K = 4
D_EXPERT = 3 * D_MODEL // 8
SWIGLU_ALPHA = 1.702
SWIGLU_LIMIT = 7.0
MOE_BLOCK = 128

F32 = jnp.float32

kernel_name = 'hybrid_s5_natten_rwkv7_gqa_moe_dit'


def rms_norm(x, g, eps=NORM_EPS):
    xf = x.astype(F32)
    xf = xf * lax.rsqrt(jnp.mean(xf * xf, axis=-1, keepdims=True) + eps)
    return (xf * g.astype(F32)).astype(x.dtype)


def split_cols(z, sizes):
    return jnp.split(z, np.cumsum(sizes)[:-1].tolist(), axis=-1)


def ctx_attention(q, k, v):
    b, l, h, e = q.shape
    hk = k.shape[2]
    qg = q.reshape(b, l, hk, h // hk, e)
    s = jnp.einsum('bqhge,bkhe->bhgqk', qg, k, preferred_element_type=F32) * (e ** -0.5)
    p = jax.nn.softmax(s, axis=-1).astype(v.dtype)
    return jnp.einsum('bhgqk,bkhe->bqhge', p, v).reshape(b, l, h, e)


def s5_discretize(lam_re, lam_im, log_dt, b_re, b_im):
    lam = lax.complex(lam_re.astype(F32), lam_im.astype(F32))
    dt = jnp.exp(log_dt.astype(F32))[:, None]
    lam_bar = jnp.exp(lam * dt)
    b = lax.complex(b_re.astype(F32), b_im.astype(F32))
    b_bar = ((lam_bar - 1.0) / lam)[..., None] * b
    return lam_bar, b_bar


def s5_scan(u, lam_bar, b_bar, h0, reverse):
    bu = jnp.einsum('btgh,gph->btgp', u.astype(jnp.complex64), b_bar)
    if h0 is not None:
        bu = bu.at[:, -1 if reverse else 0].add(lam_bar * h0)
    a = jnp.broadcast_to(lam_bar, bu.shape)

    def combine(left, right):
        a_l, b_l = left
        a_r, b_r = right
        return a_l * a_r, a_r * b_l + b_r

    _, h = lax.associative_scan(combine, (a, bu), axis=1, reverse=reverse)
    return h


def s5_readout(h, c_re, c_im):
    c = lax.complex(c_re.astype(F32), c_im.astype(F32))
    return jnp.real(jnp.einsum('btgp,ghp->btgh', h, c))


def s5_mixer(ux, uc, lam_re, lam_im, log_dt, b_re, b_im, c_re, c_im, d_skip, glu_w, glu_b, with_ctx):
    def grouped(u):
        return u.astype(F32).reshape(u.shape[0], u.shape[1], S5_GROUPS, S5_GROUP)

    def glu(y):
        g = jax.nn.gelu(y)
        return g * jax.nn.sigmoid(g @ glu_w.astype(F32) + glu_b.astype(F32))

    gx, gc = grouped(ux), grouped(uc)
    d_skip = d_skip.astype(F32)
    yx = d_skip * ux.astype(F32)
    yc = d_skip * uc.astype(F32) if with_ctx else None
    for d, rev in ((0, False), (1, True)):
        lam_bar, b_bar = s5_discretize(lam_re[d], lam_im[d], log_dt[d], b_re[d], b_im[d])
        hc = s5_scan(gc, lam_bar, b_bar, None, rev)
        hx = s5_scan(gx, lam_bar, b_bar, hc[:, 0] if rev else hc[:, -1], rev)
        yx = yx + s5_readout(hx, c_re[d], c_im[d]).reshape(yx.shape)
        if with_ctx:
            yc = yc + s5_readout(hc, c_re[d], c_im[d]).reshape(yc.shape)
    out_c = glu(yc).astype(uc.dtype) if with_ctx else None
    return glu(yx).astype(ux.dtype), out_c


def neighborhood_attention(qx, kx, vx, qc, kc, vc, rpb, with_ctx):
    bsz, seq, nh, hd = qx.shape
    rows = seq // GRID_W
    kr = min(WIN_ROWS, rows)
    scale = hd ** -0.5
    r = jnp.arange(rows)
    row_start = jnp.clip(r - kr // 2, 0, rows - kr)
    row_idx = row_start[:, None] + jnp.arange(kr)[None, :]
    col = jnp.arange(GRID_W)
    col_start = jnp.clip(col - WIN_COLS // 2, 0, GRID_W - WIN_COLS)
    col_ok = (col[None, :] >= col_start[:, None]) & (col[None, :] < col_start[:, None] + WIN_COLS)
    dr = row_idx - r[:, None] + (WIN_ROWS - 1)
    dc = jnp.clip(col[None, :] - col[:, None] + (WIN_COLS - 1), 0, 2 * WIN_COLS - 2)
    bias = rpb.astype(F32)[:, dr[:, None, :, None], dc[None, :, None, :]]
    bias = jnp.where(col_ok[None, None, :, None, :], bias, -jnp.inf)

    q = qx.reshape(bsz, rows, GRID_W, nh, hd)
    kg = kx.reshape(bsz, rows, GRID_W, nh, hd)[:, row_idx]
    vg = vx.reshape(bsz, rows, GRID_W, nh, hd)[:, row_idx]
    s_win = jnp.einsum('brqhe,brkwhe->bhrqkw', q, kg, preferred_element_type=F32) * scale + bias[None]
    s_ctx = jnp.einsum('brqhe,blhe->bhrql', q, kc, preferred_element_type=F32) * scale
    n_win = kr * GRID_W
    s = jnp.concatenate([s_win.reshape(bsz, nh, rows, GRID_W, n_win), s_ctx], axis=-1)
    p = jax.nn.softmax(s, axis=-1)
    p_win = p[..., :n_win].reshape(s_win.shape).astype(vx.dtype)
    p_ctx = p[..., n_win:].astype(vx.dtype)
    out = (jnp.einsum('bhrqkw,brkwhe->brqhe', p_win, vg)
           + jnp.einsum('bhrql,blhe->brqhe', p_ctx, vc))
    out_x = out.reshape(bsz, seq, nh * hd)
    out_c = ctx_attention(qc, kc, vc).reshape(bsz, -1, nh * hd) if with_ctx else None
    return out_x, out_c


def even_mixer(hx, hc, w_in, w_out, lam_re, lam_im, log_dt, b_re, b_im, c_re, c_im, d_skip,
               glu_w, glu_b, rpb, with_ctx):
    zx = hx @ w_in
    zc = hc @ w_in
    ux, qx, kx, vx = split_cols(zx, EVEN_SPLITS)
    uc, qc, kc, vc = split_cols(zc, EVEN_SPLITS)
    ya_x, ya_c = s5_mixer(ux, uc, lam_re, lam_im, log_dt, b_re, b_im, c_re, c_im, d_skip,
                          glu_w, glu_b, with_ctx)

    def heads(t):
        return t.reshape(t.shape[0], t.shape[1], NA_HEADS, NA_HEAD_DIM)

    yb_x, yb_c = neighborhood_attention(heads(qx), heads(kx), heads(vx), heads(qc), heads(kc),
                                        heads(vc), rpb, with_ctx)
    out_x = jnp.concatenate([ya_x, yb_x], axis=-1) @ w_out
    out_c = jnp.concatenate([ya_c, yb_c], axis=-1) @ w_out if with_ctx else None
    return out_x, out_c


def token_shift(z, mu):
    zp = jnp.pad(z, ((0, 0), (1, 0), (0, 0)))[:, :-1]
    zn = jnp.pad(z, ((0, 0), (0, 1), (0, 0)))[:, 1:]
    return z + mu[0] * (zp - z) + mu[1] * (zn - z)


def rwkv7_streams(z, mu, g_up, w0, w_up, a0, a_up, k_k, k_a):
    bsz, t, _ = z.shape
    z = token_shift(z.astype(F32), mu.astype(F32))
    r, k, v, g_lo, w_lo, a_lo = split_cols(z, RK_SPLITS)

    def heads(u):
        return u.reshape(bsz, t, RK_HEADS, RK_HEAD)

    gate = jax.nn.sigmoid(g_lo) @ g_up.astype(F32)
    w_lo = w_lo.reshape(bsz, t, 2, RK_DECAY_RANK)
    a_lo = a_lo.reshape(bsz, t, 2, RK_ICLR_RANK)
    dirs = []
    for d in range(2):
        w_log = -jax.nn.softplus(-(w0[d] + jnp.tanh(w_lo[:, :, d]) @ w_up[d])) - 0.5
        decay = jnp.exp(-jnp.exp(w_log))
        iclr = jax.nn.sigmoid(a0[d] + a_lo[:, :, d] @ a_up[d])
        kk = heads(k * k_k[d])
        kk = kk / jnp.maximum(jnp.sqrt(jnp.sum(kk * kk, axis=-1, keepdims=True)), 1e-12)
        k_d = heads(k * (1.0 + (iclr - 1.0) * k_a[d]))
        dirs.append((heads(decay), k_d, -kk, kk * heads(iclr)))
    return heads(r), heads(v), gate, dirs


def rwkv7_scan(r, w, k, v, a, b, s0, reverse):
    def step(s, inp):
        r_t, w_t, k_t, v_t, a_t, b_t = inp
        sa = jnp.einsum('bhvk,bhk->bhv', s, a_t)
        s = s * w_t[:, :, None, :] + sa[..., None] * b_t[:, :, None, :] + v_t[..., None] * k_t[:, :, None, :]
        return s, jnp.einsum('bhvk,bhk->bhv', s, r_t)

    xs = tuple(jnp.moveaxis(t, 1, 0) for t in (r, w, k, v, a, b))
    s_final, ys = lax.scan(step, s0, xs, reverse=reverse)
    return s_final, jnp.moveaxis(ys, 0, 1)


def head_group_norm(y, w, b):
    mu = jnp.mean(y, axis=-1, keepdims=True)
    var = jnp.mean(jnp.square(y - mu), axis=-1, keepdims=True)
    yn = (y - mu) * lax.rsqrt(var + RK_GN_EPS)
    return yn.reshape(y.shape[0], y.shape[1], -1) * w.astype(F32) + b.astype(F32)


def rwkv7_mixer(zx, zc, mu, g_up, w0, w_up, a0, a_up, k_k, k_a, r_k, ln_w, ln_b, with_ctx):
    params = (mu, g_up, w0, w_up, a0, a_up, k_k, k_a)
    rx, vx, gx, dirs_x = rwkv7_streams(zx, *params)
    rc, vc, gc, dirs_c = rwkv7_streams(zc, *params)
    r_k = r_k.astype(F32)
    s0 = jnp.zeros((zx.shape[0], RK_HEADS, RK_HEAD, RK_HEAD), F32)
    ys_x, bon_x, ys_c, bon_c = [], [], [], []
    for d, rev in ((0, False), (1, True)):
        w_c, k_c, a_c, b_c = dirs_c[d]
        s_ctx, y_c = rwkv7_scan(rc, w_c, k_c, vc, a_c, b_c, s0, rev)
        w_x, k_x, a_x, b_x = dirs_x[d]
        _, y_x = rwkv7_scan(rx, w_x, k_x, vx, a_x, b_x, s_ctx, rev)
        ys_x.append(y_x)
        bon_x.append(jnp.sum(rx * k_x * r_k, axis=-1, keepdims=True) * vx)
        if with_ctx:
            ys_c.append(y_c)
            bon_c.append(jnp.sum(rc * k_c * r_k, axis=-1, keepdims=True) * vc)

    def finish(ys, bons, gate):
        y = head_group_norm(ys[0] + ys[1], ln_w, ln_b) + (bons[0] + bons[1]).reshape(gate.shape)
        return y * gate

    out_x = finish(ys_x, bon_x, gx).astype(zx.dtype)
    out_c = finish(ys_c, bon_c, gc).astype(zc.dtype) if with_ctx else None
    return out_x, out_c


def rope_2d(t, row_pos, col_pos):
    e = t.shape[-1]
    half = e // 2
    inv = ROPE_THETA ** (-jnp.arange(0, half, 2, dtype=F32) / half)

    def rot(u, pos):
        ang = pos.astype(F32)[:, None] * inv[None, :]
        cos = jnp.cos(ang)[None, :, None, :]
        sin = jnp.sin(ang)[None, :, None, :]
        u1, u2 = u[..., :half // 2], u[..., half // 2:]
        return jnp.concatenate([u1 * cos - u2 * sin, u2 * cos + u1 * sin], axis=-1)

    tf = t.astype(F32)
    return jnp.concatenate([rot(tf[..., :half], row_pos), rot(tf[..., half:], col_pos)], axis=-1).astype(t.dtype)


def gqa_mixer(qx, kx, vx, qc, kc, vc, q_norm, k_norm, with_ctx):
    bsz, seq, nh, hd = qx.shape
    nkv = kx.shape[2]
    grp = nh // nkv
    pos = jnp.arange(seq)
    qx = rope_2d(rms_norm(qx, q_norm), pos // GRID_W, pos % GRID_W)
    kx = rope_2d(rms_norm(kx, k_norm), pos // GRID_W, pos % GRID_W)
    kc = rms_norm(kc, k_norm)
    k_all = jnp.concatenate([kx, kc], axis=1)
    v_all = jnp.concatenate([vx, vc], axis=1)
    n_blk = seq // Q_BLOCK
    q_blocks = jnp.moveaxis(qx.reshape(bsz, n_blk, Q_BLOCK, nkv, grp, hd), 1, 0)
    scale = hd ** -0.5

    def attend(q_blk):
        s = jnp.einsum('bqhge,bkhe->bhgqk', q_blk, k_all, preferred_element_type=F32) * scale
        p = jax.nn.softmax(s, axis=-1).astype(v_all.dtype)
        return jnp.einsum('bhgqk,bkhe->bqhge', p, v_all)

    out_x = jnp.moveaxis(lax.map(attend, q_blocks), 0, 1).reshape(bsz, seq, nh * hd)
    out_c = None
    if with_ctx:
        out_c = ctx_attention(rms_norm(qc, q_norm), kc, vc).reshape(bsz, -1, nh * hd)
    return out_x, out_c


def odd_mixer(hx, hc, w_in, w_out, mu, g_up, w0, w_up, a0, a_up, k_k, k_a, r_k, ln_w, ln_b,
              q_norm, k_norm, with_ctx):
    zx = hx @ w_in
    zc = hc @ w_in
    rkx, qx, kx, vx = split_cols(zx, ODD_SPLITS)
    rkc, qc, kc, vc = split_cols(zc, ODD_SPLITS)
    yc_x, yc_c = rwkv7_mixer(rkx, rkc, mu, g_up, w0, w_up, a0, a_up, k_k, k_a, r_k, ln_w, ln_b, with_ctx)

    def heads(t, n):
        return t.reshape(t.shape[0], t.shape[1], n, GQ_HEAD_DIM)

    yd_x, yd_c = gqa_mixer(heads(qx, GQ_HEADS), heads(kx, GQ_KV_HEADS), heads(vx, GQ_KV_HEADS),
                           heads(qc, GQ_HEADS), heads(kc, GQ_KV_HEADS), heads(vc, GQ_KV_HEADS),
                           q_norm, k_norm, with_ctx)
    out_x = jnp.concatenate([yc_x, yd_x], axis=-1) @ w_out
    out_c = jnp.concatenate([yc_c, yd_c], axis=-1) @ w_out if with_ctx else None
    return out_x, out_c


def swiglu_clamped(z):
    z_glu, z_lin = z[..., ::2], z[..., 1::2]
    z_glu = jnp.minimum(z_glu, SWIGLU_LIMIT)
    z_lin = jnp.clip(z_lin, -SWIGLU_LIMIT, SWIGLU_LIMIT)
    return z_glu * jax.nn.sigmoid(SWIGLU_ALPHA * z_glu) * (z_lin + 1.0)


def moe_ffn(h, w_router, b_router, w1, b1, w2, b2):
    n, dm = h.shape
    logits = jnp.dot(h, w_router, preferred_element_type=F32) + b_router.astype(F32)
    top_val, top_idx = lax.top_k(logits, TOP_K)
    gates = jax.nn.softmax(top_val, axis=-1)
    flat_e = top_idx.reshape(-1)
    order = jnp.argsort(flat_e)
    sorted_e = flat_e[order]
    sorted_tok = order // TOP_K
    sorted_gate = gates.reshape(-1)[order]
    counts = jnp.bincount(flat_e, length=N_EXPERTS)
    padded = (counts + MOE_BLOCK - 1) // MOE_BLOCK * MOE_BLOCK
    pad_end = jnp.cumsum(padded)
    pad_start = pad_end - padded
    grp_start = jnp.cumsum(counts) - counts
    dest = pad_start[sorted_e] + jnp.arange(n * TOP_K) - grp_start[sorted_e]
    n_blocks = -(-(n * TOP_K) // MOE_BLOCK) + N_EXPERTS
    cap = n_blocks * MOE_BLOCK
    slot_tok = jnp.full((cap,), n, jnp.int32).at[dest].set(sorted_tok.astype(jnp.int32))
    slot_gate = jnp.zeros((cap,), F32).at[dest].set(sorted_gate)
    block_exp = jnp.minimum(jnp.searchsorted(pad_end, jnp.arange(n_blocks) * MOE_BLOCK, side='right'),
                            N_EXPERTS - 1)
    h_pad = jnp.concatenate([h, jnp.zeros((1, dm), h.dtype)], axis=0)

    def expert_block(args):
        tok_blk, gate_blk, e = args
        z = h_pad[tok_blk] @ w1[e] + b1[e]
        y = swiglu_clamped(z) @ w2[e] + b2[e]
        return y.astype(F32) * gate_blk[:, None]

    yb = lax.map(expert_block, (slot_tok.reshape(n_blocks, MOE_BLOCK),
                                slot_gate.reshape(n_blocks, MOE_BLOCK), block_exp))
    y = jnp.zeros((n + 1, dm), F32).at[slot_tok].add(yb.reshape(cap, dm))
    return y[:n].astype(h.dtype)


def setup_inputs(seed: int = 0) -> dict:
    key = jax.random.key(seed)
    keys = iter(jax.random.split(key, 64))

    def nrm(shape, std):
        return jax.random.normal(next(keys), shape, F32) * std

    def uni(shape, lo, hi):
        return jax.random.uniform(next(keys), shape, F32, lo, hi)

    d = D_MODEL
    g, p, h = S5_GROUPS, S5_STATE, S5_GROUP
    return {
        'x': nrm((BATCH, SEQ, d), 1.0),
        'c': nrm((BATCH, d), 1.0),
        'ctx': nrm((BATCH, CTX_LEN, d), 1.0),
        'c_ctx': nrm((d,), 1.0),
        'w_mod': nrm((DEPTH, d, N_MOD * d), 0.5 * d ** -0.5),
        'b_mod': nrm((DEPTH, N_MOD * d), 0.02),
        'norm1': 1.0 + nrm((DEPTH, d), 0.02),
        'norm2': 1.0 + nrm((DEPTH, d), 0.02),
        'final_norm': 1.0 + nrm((d,), 0.02),
        'ev_w_in': nrm((N_EVEN, d, EVEN_IN), d ** -0.5),
        'ev_w_out': nrm((N_EVEN, 2 * MIX_W, d), (2 * MIX_W) ** -0.5),
        's5_lam_re': -0.5 + nrm((N_EVEN, 2, g, p), 0.01),
        's5_lam_im': math.pi * jnp.arange(p, dtype=F32) + nrm((N_EVEN, 2, g, p), 0.01),
        's5_log_dt': uni((N_EVEN, 2, g), math.log(S5_DT_MIN), math.log(S5_DT_MAX)),
        's5_b_re': nrm((N_EVEN, 2, g, p, h), (2 * h) ** -0.5),
        's5_b_im': nrm((N_EVEN, 2, g, p, h), (2 * h) ** -0.5),
        's5_c_re': nrm((N_EVEN, 2, g, h, p), p ** -0.5),
        's5_c_im': nrm((N_EVEN, 2, g, h, p), p ** -0.5),
        's5_d': nrm((N_EVEN, MIX_W), 0.5),
        's5_glu_w': nrm((N_EVEN, MIX_W, MIX_W), MIX_W ** -0.5),
        's5_glu_b': nrm((N_EVEN, MIX_W), 0.02),
        'na_rpb': nrm((N_EVEN, NA_HEADS, 2 * WIN_ROWS - 1, 2 * WIN_COLS - 1), 0.1),
        'od_w_in': nrm((N_ODD, d, ODD_IN), d ** -0.5),
        'od_w_out': nrm((N_ODD, 2 * MIX_W, d), (2 * MIX_W) ** -0.5),
        'rk_mu': uni((N_ODD, 2, RK_IN), 0.0, 0.5),
        'rk_g_up': nrm((N_ODD, RK_GATE_RANK, MIX_W), RK_GATE_RANK ** -0.5),
        'rk_w0': uni((N_ODD, 2, MIX_W), -6.0, -1.0),
        'rk_w_up': nrm((N_ODD, 2, RK_DECAY_RANK, MIX_W), 0.1 * RK_DECAY_RANK ** -0.5),
        'rk_a0': nrm((N_ODD, 2, MIX_W), 0.1),
        'rk_a_up': nrm((N_ODD, 2, RK_ICLR_RANK, MIX_W), 0.1 * RK_ICLR_RANK ** -0.5),
        'rk_k_k': 0.85 + nrm((N_ODD, 2, MIX_W), 0.02),
        'rk_k_a': 1.0 + nrm((N_ODD, 2, MIX_W), 0.02),
        'rk_r_k': nrm((N_ODD, RK_HEADS, RK_HEAD), 0.1),
        'rk_ln_w': 1.0 + nrm((N_ODD, MIX_W), 0.02),
        'rk_ln_b': nrm((N_ODD, MIX_W), 0.02),
        'gq_q_norm': 1.0 + nrm((N_ODD, GQ_HEAD_DIM), 0.02),
        'gq_k_norm': 1.0 + nrm((N_ODD, GQ_HEAD_DIM), 0.02),
        'moe_w_router': nrm((DEPTH, d, N_EXPERTS), d ** -0.5),
        'moe_b_router': nrm((DEPTH, N_EXPERTS), 0.01),
        'moe_w1': nrm((DEPTH, N_EXPERTS, d, 2 * D_EXPERT), d ** -0.5),
        'moe_b1': nrm((DEPTH, N_EXPERTS, 2 * D_EXPERT), 0.02),
        'moe_w2': nrm((DEPTH, N_EXPERTS, D_EXPERT, d), D_EXPERT ** -0.5),
        'moe_b2': nrm((DEPTH, N_EXPERTS, d), 0.02),
    }


def reference(x, c, ctx, c_ctx, w_mod, b_mod, norm1, norm2, final_norm,
              ev_w_in, ev_w_out, s5_lam_re, s5_lam_im, s5_log_dt, s5_b_re, s5_b_im, s5_c_re, s5_c_im,
              s5_d, s5_glu_w, s5_glu_b, na_rpb,
              od_w_in, od_w_out, rk_mu, rk_g_up, rk_w0, rk_w_up, rk_a0, rk_a_up, rk_k_k, rk_k_a, rk_r_k,
              rk_ln_w, rk_ln_b, gq_q_norm, gq_k_norm,
              moe_w_router, moe_b_router, moe_w1, moe_b1, moe_w2, moe_b2):
    bsz, seq, dm = x.shape
    n_ctx = ctx.shape[1]
    h_x, h_c = x, ctx
    cond_x = jax.nn.silu(c)
    cond_c = jax.nn.silu(c_ctx)
    for i in range(DEPTH):
        last = i == DEPTH - 1
        j = i // 2
        mod_x = (cond_x @ w_mod[i] + b_mod[i])[:, None, :]
        mod_c = cond_c @ w_mod[i] + b_mod[i]
        sh1x, sc1x, g1x, sh2x, sc2x, g2x = jnp.split(mod_x, N_MOD, axis=-1)
        sh1c, sc1c, g1c, sh2c, sc2c, g2c = jnp.split(mod_c, N_MOD, axis=-1)
        ax = rms_norm(h_x, norm1[i]) * (1.0 + sc1x) + sh1x
        ac = rms_norm(h_c, norm1[i]) * (1.0 + sc1c) + sh1c
        if i % 2 == 0:
            ox, oc = even_mixer(ax, ac, ev_w_in[j], ev_w_out[j], s5_lam_re[j], s5_lam_im[j], s5_log_dt[j],
                                s5_b_re[j], s5_b_im[j], s5_c_re[j], s5_c_im[j], s5_d[j], s5_glu_w[j],
                                s5_glu_b[j], na_rpb[j], not last)
        else:
            ox, oc = odd_mixer(ax, ac, od_w_in[j], od_w_out[j], rk_mu[j], rk_g_up[j], rk_w0[j], rk_w_up[j],
                               rk_a0[j], rk_a_up[j], rk_k_k[j], rk_k_a[j], rk_r_k[j], rk_ln_w[j], rk_ln_b[j],
                               gq_q_norm[j], gq_k_norm[j], not last)
        h_x = h_x + g1x * ox
        fx = (rms_norm(h_x, norm2[i]) * (1.0 + sc2x) + sh2x).reshape(bsz * seq, dm)
        moe_args = (moe_w_router[i], moe_b_router[i], moe_w1[i], moe_b1[i], moe_w2[i], moe_b2[i])
        if last:
            y = moe_ffn(fx, *moe_args)
            h_x = h_x + g2x * y.reshape(bsz, seq, dm)
        else:
            h_c = h_c + g1c * oc
            fc = (rms_norm(h_c, norm2[i]) * (1.0 + sc2c) + sh2c).reshape(bsz * n_ctx, dm)
            y = moe_ffn(jnp.concatenate([fx, fc], axis=0), *moe_args)
            h_x = h_x + g2x * y[:bsz * seq].reshape(bsz, seq, dm)
            h_c = h_c + g2c * y[bsz * seq:].reshape(bsz, n_ctx, dm)
    return rms_norm(h_x, final_norm)
```

```python
import contextlib
import numpy as np
import concourse.bass as bass
import concourse.mybir as mybir
from concourse.bass_utils import run_bass_kernel_spmd

F32 = mybir.dt.float32
BF16 = mybir.dt.bfloat16
ALU = mybir.AluOpType
AF = mybir.ActivationFunctionType
AX = mybir.AxisListType

NCORES = 8
D = 2048
KC = 16
LCTX = 256
SEQ = 4096
T = LCTX + SEQ
DEPTH = 4


class Sched:
    def __init__(self, nc, stack, n_dma_sems=12):
        self.nc = nc
        self.E = {'pe': nc.tensor, 'dve': nc.vector, 'act': nc.scalar,
                  'pool': nc.gpsimd, 'sp': nc.sync}
        self.sem = {}
        self.cnt = {}
        for e in ('pe', 'dve', 'act', 'pool'):
            self.sem[e] = stack.enter_context(nc.semaphore('sem_' + e))
            self.cnt[e] = 0
        self.dsem = [stack.enter_context(nc.semaphore('dsem%d' % i)) for i in range(n_dma_sems)]
        self.dcnt = [0] * n_dma_sems
        self.dn = 0
        self.waited = {}
        self.lastw = {}
        self.readers = {}
        self.self_sync = {'pe': False, 'dve': True, 'act': True, 'pool': True, 'sp': True}

    def _wait(self, eng, sem, val):
        if self.waited.get((eng, sem), 0) < val:
            self.E[eng].wait_ge(sem, val)
            self.waited[(eng, sem)] = val

    @staticmethod
    def _key(b):
        if isinstance(b, (str, int)):
            return b
        if isinstance(b, tuple):
            return tuple(Sched._key(x) for x in b)
        return b.name

    def _deps(self, eng, reads, writes):
        reads = [self._key(b) for b in reads]
        writes = [self._key(b) for b in writes]
        need = {}

        def add(ev):
            sem, val = ev
            if val > need.get(sem, 0):
                need[sem] = val
        for b in reads:
            if b in self.lastw:
                add(self.lastw[b])
        for b in writes:
            if b in self.lastw:
                add(self.lastw[b])
            for sem, val in self.readers.get(b, {}).items():
                add((sem, val))
        own = self.sem.get(eng)
        for sem, val in need.items():
            if sem is own and not self.self_sync[eng]:
                continue
            self._wait(eng, sem, val)

    def _commit(self, ev, reads, writes):
        reads = [self._key(b) for b in reads]
        writes = [self._key(b) for b in writes]
        sem, val = ev
        for b in reads:
            d = self.readers.setdefault(b, {})
            if d.get(sem, 0) < val:
                d[sem] = val
        for b in writes:
            self.lastw[b] = ev
            self.readers[b] = {}

    def op(self, eng, reads, writes, fn):
        self._deps(eng, reads, writes)
        ins = fn(self.E[eng])
        self.cnt[eng] += 1
        ins.then_inc(self.sem[eng], 1)
        self._commit((self.sem[eng], self.cnt[eng]), reads, writes)

    def dma(self, out, in_, reads, writes, eng='sp', **kw):
        k = self.dn % len(self.dsem)
        self.dn += 1
        sem = self.dsem[k]
        if self.dcnt[k] > 0:
            self._wait(eng, sem, self.dcnt[k])
        self._deps(eng, reads, writes)
        self.E[eng].dma_start(out=out, in_=in_, **kw).then_inc(sem, 16)
        self.dcnt[k] += 16
        self._commit((sem, self.dcnt[k]), reads, writes)

    def barrier(self):
        for e in ('pe', 'dve', 'act', 'pool', 'sp'):
            for e2 in ('pe', 'dve', 'act', 'pool'):
                if self.cnt[e2] > 0:
                    self._wait(e, self.sem[e2], self.cnt[e2])
            for k, sem in enumerate(self.dsem):
                if self.dcnt[k] > 0:
                    self._wait(e, sem, self.dcnt[k])
        self.lastw.clear()
        self.readers.clear()

    def finish(self, eng='sp'):
        for k, sem in enumerate(self.dsem):
            if self.dcnt[k] > 0:
                self._wait(eng, sem, self.dcnt[k])
        for e in ('pe', 'dve', 'act', 'pool'):
            if self.cnt[e] > 0:
                self._wait(eng, self.sem[e], self.cnt[e])


class Ring:
    def __init__(self, nc, stack, name, shape, dtype, n, psum=False):
        self.tiles = []
        for i in range(n):
            if psum:
                t = stack.enter_context(nc.psum_tensor('%s%s%d' % (_PFX[0], name, i), list(shape), dtype))
            else:
                t = stack.enter_context(nc.sbuf_tensor('%s%s%d' % (_PFX[0], name, i), list(shape), dtype))
            self.tiles.append(t)
        self.i = 0

    def get(self):
        t = self.tiles[self.i % len(self.tiles)]
        self.i += 1
        return t


_PFX = ['']


def sb(nc, stack, name, shape, dtype=F32):
    return stack.enter_context(nc.sbuf_tensor(_PFX[0] + name, list(shape), dtype))


def run_spmd(nc, in_maps):
    res = run_bass_kernel_spmd(nc, in_maps, core_ids=list(range(len(in_maps))))
    return res.results


def emit_mod(nc, S, io, nl=DEPTH):
    NM = 6 * D
    cT = io.inp("cT", [128, KC, 2])
    w_mod = io.inp("w_mod", [nl, D, NM])
    b_mod = io.inp("b_mod", [nl, NM])
    out = io.out("mod", [nl, 2, NM])
    CB = 512
    with contextlib.ExitStack() as st:
        c_sb = sb(nc, st, "c_sb", [128, KC, 2])
        cs_sb = sb(nc, st, "cs_sb", [128, KC, 2])
        bias = sb(nc, st, "bias", [2, NM])
        res = sb(nc, st, "res", [2, NM])
        wr = Ring(nc, st, "w", [128, KC, CB], F32, 3)
        pr = Ring(nc, st, "ps", [2, CB], F32, 2, psum=True)
        S.dma(c_sb[:], cT, [], [c_sb])
        S.op('act', [c_sb], [cs_sb], lambda e: e.activation(cs_sb[:], c_sb[:], AF.Silu))
        for i in range(nl):
            for r in range(2):
                S.dma(bias[r:r + 1, :], b_mod[i:i + 1, :], [], [bias])
            for cb in range(NM // CB):
                w = wr.get()
                S.dma(w[:], w_mod[i, :, cb * CB:(cb + 1) * CB].rearrange("(k p) n -> p k n", p=128), [], [w])
                ps = pr.get()

                def mm(e, w=w, ps=ps):
                    ins = None
                    for k in range(KC):
                        ins = e.matmul(ps[:], cs_sb[:, k, :], w[:, k, :], start=(k == 0), stop=(k == KC - 1))
                    return ins
                S.op('pe', [w, cs_sb], [ps], mm)
                S.op('dve', [ps, bias], [res],
                     lambda e, ps=ps, cb=cb: e.tensor_tensor(res[:, cb * CB:(cb + 1) * CB], ps[:],
                                                             bias[:, cb * CB:(cb + 1) * CB], ALU.add))
            S.dma(out[i], res[:], [res], [])
        S.barrier()


def run_mod(c, c_ctx, w_mod, b_mod):
    nc = build_mod()
    in_maps = []
    for b in range(NCORES):
        cc = np.stack([c[b], c_ctx], axis=-1)
        cT = np.ascontiguousarray(cc.reshape(KC, 128, 2).transpose(1, 0, 2))
        in_maps.append({"cT": cT, "w_mod": w_mod, "b_mod": b_mod})
    res = run_spmd(nc, in_maps)
    return [r["mod"] for r in res]


TOK_BLOCKS = [(0, LCTX, 1)] + [(LCTX + 512 * i, 512, 0) for i in range(SEQ // 512)]
EPS = 1e-6


def emit_norm_mod(S, nc, h_sb, n, col, geff, shf, ones, sq, ss_ps, rstd, tmp, aT):
    for k in range(KC):
        sqk = sq.get()
        S.op('act', [h_sb], [sqk], lambda e, k=k, sqk=sqk: e.activation(sqk[:, :n], h_sb[:, k, :n], AF.Square))
        S.op('pe', [sqk, ones], [ss_ps],
             lambda e, k=k, sqk=sqk: e.matmul(ss_ps[:, :n], ones[:], sqk[:, :n], start=(k == 0), stop=(k == KC - 1)))
    S.op('dve', [ss_ps], [rstd], lambda e: e.tensor_scalar(rstd[:, :n], ss_ps[:, :n], 1.0 / D, EPS, ALU.mult, ALU.add))
    S.op('act', [rstd], [rstd], lambda e: e.activation(rstd[:, :n], rstd[:, :n], AF.Sqrt))
    S.op('dve', [rstd], [rstd], lambda e: e.reciprocal(rstd[:, :n], rstd[:, :n]))
    for k in range(KC):
        tk = tmp.get()
        S.op('dve', [h_sb, rstd, geff], [tk],
             lambda e, k=k, tk=tk: e.scalar_tensor_tensor(tk[:, :n], h_sb[:, k, :n], geff[:, k, col:col + 1], rstd[:, :n],
                                                          ALU.mult, ALU.mult))
        S.op('act', [tk, shf], [(aT, k)],
             lambda e, k=k, tk=tk: e.activation(aT[:, k, :n], tk[:, :n], AF.Identity, bias=shf[:, k, col:col + 1], scale=1.0))


def emit_geff(S, nc, st, normT_d, scT_d, shT_d, name):
    nw = sb(nc, st, name + "_nw", [128, KC])
    sc = sb(nc, st, name + "_sc", [128, KC, 2])
    shf = sb(nc, st, name + "_sh", [128, KC, 2])
    geff = sb(nc, st, name + "_ge", [128, KC, 2])
    S.dma(nw[:], normT_d, [], [nw])
    for c_ in range(2):
        S.dma(sc[:, :, c_], scT_d[:, :, c_], [], [sc], allow_slow_non_contiguous=True)
        S.dma(shf[:, :, c_], shT_d[:, :, c_], [], [shf], allow_slow_non_contiguous=True)
    S.op('dve', [sc], [sc], lambda e: e.tensor_scalar_add(sc[:], sc[:], 1.0))
    for c in range(2):
        S.op('dve', [sc, nw], [geff], lambda e, c=c: e.tensor_tensor(geff[:, :, c], sc[:, :, c], nw[:], ALU.mult))
    return geff, shf


def emit_linear_block(S, nc, R, aT, n, kc, w_d, C, evac, vrange=None, evac_tm=None):
    CG = 512
    for c0 in range(0, C, CG):
        cw = min(CG, C - c0)
        w32 = R['w32'].get()
        S.dma(w32[:, :kc, :cw], w_d[:, c0:c0 + cw].rearrange("(k p) n -> p k n", p=128), [], [w32])
        wbf = R['wbf'].get()
        ceng = ('dve', 'pool')[(c0 // CG) % 2]
        S.op(ceng, [w32], [wbf], lambda e: e.tensor_copy(wbf[:, :kc, :cw], w32[:, :kc, :cw]))
        for m0 in range(0, cw, 128):
            col = c0 + m0
            if vrange is not None and vrange[0] <= col < vrange[1]:
                for tb in range(n // 128):
                    ps = R['ps'].get()

                    def mmt(e, ps=ps, m0=m0, tb=tb):
                        ins = None
                        for k in range(kc):
                            ins = e.matmul(ps[:, :128], aT[:, k, tb * 128:(tb + 1) * 128], wbf[:, k, m0:m0 + 128], start=(k == 0), stop=(k == kc - 1))
                        return ins
                    S.op('pe', [wbf] + [(aT, k) for k in range(kc)], [ps], mmt)
                    evac_tm(col - vrange[0], tb, ps)
                continue
            ps = R['ps'].get()

            def mm(e, ps=ps, m0=m0):
                ins = None
                for k in range(kc):
                    ins = e.matmul(ps[:, :n], wbf[:, k, m0:m0 + 128], aT[:, k, :n], start=(k == 0), stop=(k == kc - 1))
                return ins
            S.op('pe', [wbf] + [(aT, k) for k in range(kc)], [ps], mm)
            evac(col // 128, ps)


def emit_p1(nc, S, io, C, vrange=None):
    hT = io.inp("hT", [D, T])
    normT = io.inp("normT", [128, KC])
    scT = io.inp("scT", [128, KC, 2])
    shT = io.inp("shT", [128, KC, 2])
    w_in = io.inp("w_in", [D, C])
    zT = io.out("zT", [C, T])
    v_tm = io.out("v_tm", [T, vrange[1] - vrange[0]]) if vrange is not None else None
    with contextlib.ExitStack() as st:
        ones = sb(nc, st, "ones", [128, 128])
        S.op('pool', [], [ones], lambda e: e.memset(ones[:], 1.0))
        geff, shf = emit_geff(S, nc, st, normT, scT, shT, "n1")
        hr = Ring(nc, st, "h", [128, KC, 512], F32, 1)
        sq = Ring(nc, st, "sq", [128, 512], F32, 3)
        tmp = Ring(nc, st, "tmp", [128, 512], F32, 3)
        rstd = sb(nc, st, "rstd", [128, 512])
        ar = Ring(nc, st, "aT", [128, KC, 512], BF16, 2)
        R = {'w32': Ring(nc, st, "w32", [128, KC, 512], F32, 2),
             'wbf': Ring(nc, st, "wbf", [128, KC, 512], BF16, 2),
             'ps': Ring(nc, st, "ps", [128, 512], F32, 4, psum=True)}
        ss_ps = st.enter_context(nc.psum_tensor(_PFX[0] + "ss_ps", [128, 512], F32))
        zr = Ring(nc, st, "z", [128, 512], F32, 4)
        hTv = hT.rearrange("(k p) t -> p k t", p=128)
        cnt = [0]
        for (t0, n, col) in TOK_BLOCKS:
            h_sb = hr.get()
            S.dma(h_sb[:, :, :n], hTv[:, :, t0:t0 + n], [], [h_sb])
            aT = ar.get()
            emit_norm_mod(S, nc, h_sb, n, col, geff, shf, ones, sq, ss_ps, rstd, tmp, aT)

            def evac(m, ps, t0=t0, n=n):
                z = zr.get()
                eng = ('act', 'dve')[cnt[0] % 2]
                cnt[0] += 1
                if eng == 'act':
                    S.op('act', [ps], [z], lambda e: e.copy(z[:, :n], ps[:, :n]))
                else:
                    S.op('dve', [ps], [z], lambda e: e.tensor_copy(z[:, :n], ps[:, :n]))
                S.dma(zT[m * 128:(m + 1) * 128, t0:t0 + n], z[:, :n], [z], [])

            def evac_tm(c_off, tb, ps, t0=t0):
                z = zr.get()
                eng = ('act', 'dve')[cnt[0] % 2]
                cnt[0] += 1
                if eng == 'act':
                    S.op('act', [ps], [z], lambda e: e.copy(z[:, :128], ps[:, :128]))
                else:
                    S.op('dve', [ps], [z], lambda e: e.tensor_copy(z[:, :128], ps[:, :128]))
                S.dma(v_tm[t0 + tb * 128:t0 + (tb + 1) * 128, c_off:c_off + 128], z[:, :128], [z], [])
            emit_linear_block(S, nc, R, aT, n, KC, w_in, C, evac, vrange, evac_tm)
        S.barrier()


def fm(v):
    v = np.asarray(v)
    if v.ndim == 1:
        return np.ascontiguousarray(v.reshape(-1, 128).T)
    return np.ascontiguousarray(v.reshape(-1, 128, v.shape[-1]).transpose(1, 0, 2))


def mod_parts(mod_i):
    parts = []
    for j in range(6):
        seg = mod_i[:, j * D:(j + 1) * D]
        parts.append(fm(seg.T))
    return parts


NE = 32
DE = 768
HC = DE // 128


def emit_p6(nc, S, io, n_experts=NE, skip_ctx=False):
    di = io.inp
    hT = di("hT", [D, T])
    yT = di("yT", [D, T])
    w_out = di("w_out", [D, D])
    g1T = di("g1T", [128, KC, 2])
    normT = di("normT", [128, KC])
    scT = di("scT", [128, KC, 2])
    shT = di("shT", [128, KC, 2])
    g2T = di("g2T", [128, KC, 2])
    w_r = di("w_r", [D, NE])
    b_r = di("b_r", [128, NE])
    w1 = di("w1", [NE, D, 2 * DE])
    b1g = di("b1g", [128, NE, HC])
    b1l = di("b1l", [128, NE, HC])
    w2 = di("w2", [NE, DE, D])
    b2 = di("b2", [NE, D])
    ident_d = di("ident", [128, 128])
    sel_d = di("sel", [NE, NE * 128])
    hoT = io.out("hoT", [D, T])
    with contextlib.ExitStack() as st:
        ones = sb(nc, st, "ones", [128, 128])
        S.op('pool', [], [ones], lambda e: e.memset(ones[:], 1.0))
        ident = sb(nc, st, "ident_s", [128, 128])
        S.dma(ident[:], ident_d, [], [ident])
        sel = sb(nc, st, "sel_s", [NE, NE * 128])
        S.dma(sel[:], sel_d, [], [sel])
        g1 = sb(nc, st, "g1", [128, KC, 2])
        for c_ in range(2):
            S.dma(g1[:, :, c_], g1T[:, :, c_], [], [g1], allow_slow_non_contiguous=True)
        g2 = sb(nc, st, "g2", [128, KC, 2])
        for c_ in range(2):
            S.dma(g2[:, :, c_], g2T[:, :, c_], [], [g2], allow_slow_non_contiguous=True)
        wr_sb = sb(nc, st, "wr_sb", [128, KC, NE])
        S.dma(wr_sb[:], w_r.rearrange("(k p) n -> p k n", p=128), [], [wr_sb])
        br_sb = sb(nc, st, "br_sb", [128, NE])
        S.dma(br_sb[:], b_r, [], [br_sb])
        b1g_sb = sb(nc, st, "b1g_sb", [128, NE, HC])
        S.dma(b1g_sb[:], b1g, [], [b1g_sb])
        b1l_sb = sb(nc, st, "b1l_sb", [128, NE, HC])
        S.dma(b1l_sb[:], b1l, [], [b1l_sb])
        b2_sb = sb(nc, st, "b2_sb", [NE, D])
        S.dma(b2_sb[:], b2, [], [b2_sb])
        geff, shf = emit_geff(S, nc, st, normT, scT, shT, "n2")

        h_sb = sb(nc, st, "h_sb", [128, KC, 512])
        yacc = sb(nc, st, "yacc", [128, KC, 512])
        aT = sb(nc, st, "aT", [128, KC, 512], BF16)
        stg = Ring(nc, st, "stg", [128, 512], F32, 3)
        sq = Ring(nc, st, "sq", [128, 512], F32, 2)
        tmp = Ring(nc, st, "tmp", [128, 512], F32, 2)
        rstd = sb(nc, st, "rstd", [128, 512])
        w32r = Ring(nc, st, "w32", [128, 4096], F32, 2)
        wbfr = Ring(nc, st, "wbf", [128, 4096], BF16, 2)
        actr = Ring(nc, st, "act", [128, HC, 512], BF16, 2)
        gr = Ring(nc, st, "ggr", [128, 512], F32, 2)
        sr = Ring(nc, st, "ssr", [128, 512], F32, 2)
        lr = Ring(nc, st, "llr", [128, 512], F32, 2)
        zr = stg
        lg = sb(nc, st, "lg", [128, NE])
        mx8 = sb(nc, st, "mx8", [128, 8])
        nmx = sb(nc, st, "nmx", [128, 1])
        msk = sb(nc, st, "msk", [128, NE])
        ex = sb(nc, st, "ex", [128, NE])
        ssum = sb(nc, st, "ssum", [128, 1])
        gts = sb(nc, st, "gts", [128, NE])
        gT = sb(nc, st, "gT", [NE, 512])
        psr = Ring(nc, st, "ps", [128, 512], F32, 4, psum=True)
        ss_ps = st.enter_context(nc.psum_tensor(_PFX[0] + "ss_ps", [128, 512], F32))
        gbr = Ring(nc, st, "gb", [128, 512], F32, 2, psum=True)
        sm_ps = st.enter_context(nc.psum_tensor(_PFX[0] + "sm_ps", [128, 512], F32))

        hTv = hT.rearrange("(k p) t -> p k t", p=128)
        yTv = yT.rearrange("(k p) t -> p k t", p=128)
        hoTv = hoT.rearrange("(k p) t -> p k t", p=128)
        cnt = [0]
        for (t0, n, col) in (TOK_BLOCKS[1:] if skip_ctx else TOK_BLOCKS):
            S.dma(h_sb[:, :, :n], hTv[:, :, t0:t0 + n], [], [h_sb] + [(h_sb, m) for m in range(KC)])
            for k in range(KC):
                sg = stg.get()
                S.dma(sg[:, :n], yTv[:, k, t0:t0 + n], [], [sg])
                eng = ('dve', 'act')[k % 2]
                if eng == 'dve':
                    S.op('dve', [sg], [(aT, k)], lambda e, k=k, sg=sg: e.tensor_copy(aT[:, k, :n], sg[:, :n]))
                else:
                    S.op('act', [sg], [(aT, k)], lambda e, k=k, sg=sg: e.copy(aT[:, k, :n], sg[:, :n]))
            R = {'w32': w32r, 'wbf': wbfr, 'ps': psr}

            def evac_o(m, ps, n=n, col=col):
                S.op('dve', [ps, g1, (h_sb, m)], [(h_sb, m)],
                     lambda e: e.scalar_tensor_tensor(h_sb[:, m, :n], ps[:, :n], g1[:, m, col:col + 1], h_sb[:, m, :n],
                                                      ALU.mult, ALU.add))
            emit_linear_block_flat(S, nc, R, aT, n, KC, w_out, D, evac_o)
            hk = [(h_sb, m) for m in range(KC)]
            for k in range(KC):
                sqk = sq.get()
                S.op('act', hk, [sqk], lambda e, k=k, sqk=sqk: e.activation(sqk[:, :n], h_sb[:, k, :n], AF.Square))
                S.op('pe', [sqk, ones], [ss_ps],
                     lambda e, k=k, sqk=sqk: e.matmul(ss_ps[:, :n], ones[:], sqk[:, :n], start=(k == 0), stop=(k == KC - 1)))
            S.op('dve', [ss_ps], [rstd], lambda e: e.tensor_scalar(rstd[:, :n], ss_ps[:, :n], 1.0 / D, EPS, ALU.mult, ALU.add))
            S.op('act', [rstd], [rstd], lambda e: e.activation(rstd[:, :n], rstd[:, :n], AF.Sqrt))
            S.op('dve', [rstd], [rstd], lambda e: e.reciprocal(rstd[:, :n], rstd[:, :n]))
            for k in range(KC):
                tk = tmp.get()
                S.op('dve', hk + [rstd, geff], [tk],
                     lambda e, k=k, tk=tk: e.scalar_tensor_tensor(tk[:, :n], h_sb[:, k, :n], geff[:, k, col:col + 1],
                                                                  rstd[:, :n], ALU.mult, ALU.mult))
                S.op('act', [tk, shf], [(yacc, k)],
                     lambda e, k=k, tk=tk: e.activation(yacc[:, k, :n], tk[:, :n], AF.Identity,
                                                        bias=shf[:, k, col:col + 1], scale=1.0))
                S.op('dve', [(yacc, k)], [(aT, k)], lambda e, k=k: e.tensor_copy(aT[:, k, :n], yacc[:, k, :n]))
            for tb in range(n // 128):
                def mmr(e, tb=tb):
                    ins = None
                    for k in range(KC):
                        ins = e.matmul(sm_ps[:, :NE], yacc[:, k, tb * 128:(tb + 1) * 128], wr_sb[:, k, :],
                                       start=(k == 0), stop=(k == KC - 1))
                    return ins
                S.op('pe', [(yacc, k) for k in range(KC)] + [wr_sb], [sm_ps], mmr)
                S.op('dve', [sm_ps, br_sb], [lg], lambda e: e.tensor_tensor(lg[:], sm_ps[:, :NE], br_sb[:], ALU.add))
                S.op('dve', [lg], [mx8], lambda e: e.max(mx8[:], lg[:]))
                S.op('dve', [lg, mx8], [msk], lambda e: e.tensor_scalar(msk[:], lg[:], mx8[:, 3:4], None, ALU.is_ge))
                S.op('dve', [mx8], [nmx], lambda e: e.tensor_scalar_mul(nmx[:], mx8[:, 0:1], -1.0))
                S.op('act', [lg, nmx], [ex], lambda e: e.activation(ex[:], lg[:], AF.Exp, bias=nmx[:, 0:1], scale=1.0))
                S.op('dve', [ex, msk], [ex], lambda e: e.tensor_tensor(ex[:], ex[:], msk[:], ALU.mult))
                S.op('dve', [ex], [ssum], lambda e: e.reduce_sum(ssum[:], ex[:], AX.X))
                S.op('dve', [ssum], [ssum], lambda e: e.reciprocal(ssum[:], ssum[:]))
                S.op('dve', [ex, ssum], [gts], lambda e: e.tensor_scalar_mul(gts[:], ex[:], ssum[:, 0:1]))
                S.op('pe', [gts, ident], [sm_ps], lambda e: e.transpose(sm_ps[:NE, :128], gts[:], ident[:]))
                S.op('act', [sm_ps], [gT], lambda e, tb=tb: e.copy(gT[:, tb * 128:(tb + 1) * 128], sm_ps[:NE, :128]))
            for m in range(KC):
                ps = psr.get()
                S.op('pe', [b2_sb, gT], [ps],
                     lambda e, m=m, ps=ps: e.matmul(ps[:, :n], b2_sb[:, m * 128:(m + 1) * 128], gT[:, :n], start=True, stop=True))
                S.op('act', [ps], [(yacc, m)], lambda e, m=m, ps=ps: e.copy(yacc[:, m, :n], ps[:, :n]))
            for ex_i in range(n_experts):
                gb = gbr.get()
                S.op('pe', [sel, gT], [gb],
                     lambda e, gb=gb, ex_i=ex_i: e.matmul(gb[:, :n], sel[:, ex_i * 128:(ex_i + 1) * 128], gT[:, :n],
                                                          start=True, stop=True))
                act = actr.get()
                for j in range(HC):
                    w32 = w32r.get()
                    S.dma(w32[:].rearrange("p (k c) -> p k c", k=KC),
                          w1[ex_i, :, j * 256:(j + 1) * 256].rearrange("(k p) c -> p k c", p=128), [], [w32])
                    wbf = wbfr.get()
                    S.op('pool', [w32], [wbf],
                         lambda e, w32=w32, wbf=wbf: e.tensor_copy(
                             wbf[:].rearrange("p (k two c) -> p k two c", k=KC, two=2),
                             w32[:].rearrange("p (k c two) -> p k two c", k=KC, two=2)))
                    wv = wbf[:].rearrange("p (k two c) -> p k two c", k=KC, two=2)
                    psg = psr.get()
                    psl = psr.get()

                    def mm1(e, wv=wv, psg=psg, psl=psl):
                        ins = None
                        for k in range(KC):
                            e.matmul(psg[:, :n], wv[:, k, 0, :], aT[:, k, :n], start=(k == 0), stop=(k == KC - 1))
                        for k in range(KC):
                            ins = e.matmul(psl[:, :n], wv[:, k, 1, :], aT[:, k, :n], start=(k == 0), stop=(k == KC - 1))
                        return ins
                    S.op('pe', [wbf] + [(aT, k) for k in range(KC)], [psg, psl], mm1)
                    g_t = gr.get()
                    s_t = sr.get()
                    l_t = lr.get()
                    S.op('dve', [psg, b1g_sb], [g_t],
                         lambda e, g_t=g_t, psg=psg, j=j, ex_i=ex_i: e.tensor_scalar(
                             g_t[:, :n], psg[:, :n], b1g_sb[:, ex_i, j:j + 1], 7.0, ALU.add, ALU.min))
                    S.op('act', [g_t], [s_t],
                         lambda e, g_t=g_t, s_t=s_t: e.activation(s_t[:, :n], g_t[:, :n], AF.Sigmoid, scale=1.702))
                    S.op('dve', [psl, b1l_sb], [l_t],
                         lambda e, l_t=l_t, psl=psl, j=j, ex_i=ex_i: e.tensor_scalar(
                             l_t[:, :n], psl[:, :n], b1l_sb[:, ex_i, j:j + 1], 7.0, ALU.add, ALU.min))
                    S.op('pool', [l_t], [l_t],
                         lambda e, l_t=l_t: e.tensor_scalar(l_t[:, :n], l_t[:, :n], -7.0, 1.0, ALU.max, ALU.add))
                    S.op('pool', [g_t, s_t], [g_t], lambda e, g_t=g_t, s_t=s_t: e.tensor_tensor(g_t[:, :n], g_t[:, :n], s_t[:, :n], ALU.mult))
                    S.op('dve', [g_t, l_t], [g_t], lambda e, g_t=g_t, l_t=l_t: e.tensor_tensor(g_t[:, :n], g_t[:, :n], l_t[:, :n], ALU.mult))
                    S.op('dve', [g_t, gb], [(act, j)],
                         lambda e, g_t=g_t, gb=gb, act=act, j=j: e.tensor_tensor(act[:, j, :n], g_t[:, :n], gb[:, :n], ALU.mult))
                for cg in range(4):
                    w32 = w32r.get()
                    S.dma(w32[:, :HC * 512].rearrange("p (k c) -> p k c", k=HC),
                          w2[ex_i, :, cg * 512:(cg + 1) * 512].rearrange("(k p) c -> p k c", p=128), [], [w32])
                    wbf = wbfr.get()
                    ceng = ('act', 'dve')[cg % 2]
                    if ceng == 'act':
                        S.op('act', [w32], [wbf], lambda e, w32=w32, wbf=wbf: e.copy(wbf[:, :HC * 512], w32[:, :HC * 512]))
                    else:
                        S.op('dve', [w32], [wbf], lambda e, w32=w32, wbf=wbf: e.tensor_copy(wbf[:, :HC * 512], w32[:, :HC * 512]))
                    wv2 = wbf[:, :HC * 512].rearrange("p (k c) -> p k c", k=HC)
                    for mm_i in range(4):
                        m = cg * 4 + mm_i
                        ps = psr.get()

                        def mm2(e, ps=ps, wv2=wv2, mm_i=mm_i, act=act):
                            ins = None
                            for k in range(HC):
                                ins = e.matmul(ps[:, :n], wv2[:, k, mm_i * 128:(mm_i + 1) * 128], act[:, k, :n],
                                               start=(k == 0), stop=(k == HC - 1))
                            return ins
                        S.op('pe', [wbf] + [(act, k) for k in range(HC)], [ps], mm2)
                        S.op('dve', [ps, (yacc, m)], [(yacc, m)],
                             lambda e, ps=ps, m=m: e.tensor_tensor(yacc[:, m, :n], yacc[:, m, :n], ps[:, :n], ALU.add))
            for m in range(KC):
                z = zr.get()
                S.op('dve', [(yacc, m), (h_sb, m), g2], [z],
                     lambda e, z=z, m=m: e.scalar_tensor_tensor(z[:, :n], yacc[:, m, :n], g2[:, m, col:col + 1], h_sb[:, m, :n],
                                                                ALU.mult, ALU.add))
                S.dma(hoTv[:, m, t0:t0 + n], z[:, :n], [z], [])
        S.barrier()


def emit_linear_block_flat(S, nc, R, aT, n, kc, w_d, C, evac):
    CG = 4096 // kc
    for c0 in range(0, C, CG):
        cw = min(CG, C - c0)
        w32 = R['w32'].get()
        S.dma(w32[:, :kc * cw].rearrange("p (k c) -> p k c", k=kc),
              w_d[:, c0:c0 + cw].rearrange("(k p) n -> p k n", p=128), [], [w32])
        wbf = R['wbf'].get()
        ceng = ('dve', 'pool')[(c0 // CG) % 2]
        S.op(ceng, [w32], [wbf], lambda e, w32=w32, wbf=wbf: e.tensor_copy(wbf[:, :kc * cw], w32[:, :kc * cw]))
        wv = wbf[:, :kc * cw].rearrange("p (k c) -> p k c", k=kc)
        for m0 in range(0, cw, 128):
            ps = R['ps'].get()

            def mm(e, ps=ps, m0=m0, wv=wv):
                ins = None
                for k in range(kc):
                    ins = e.matmul(ps[:, :n], wv[:, k, m0:m0 + 128], aT[:, k, :n], start=(k == 0), stop=(k == kc - 1))
                return ins
            S.op('pe', [wbf] + [(aT, k) for k in range(kc)], [ps], mm)
            evac((c0 + m0) // 128, ps)


def p6_inputs(z, i, parts, hT, yT):
    j = i // 2
    w_out = z['ev_w_out'][j] if i % 2 == 0 else z['od_w_out'][j]
    b1 = z['moe_b1'][i]
    b1g = np.ascontiguousarray(b1[:, 0::2].reshape(NE, HC, 128).transpose(2, 0, 1))
    b1l = np.ascontiguousarray(b1[:, 1::2].reshape(NE, HC, 128).transpose(2, 0, 1))
    sel = np.zeros((NE, NE, 128), np.float32)
    for e in range(NE):
        sel[e, e, :] = 1.0
    return {"hT": hT, "yT": yT, "w_out": w_out, "g1T": parts[2], "normT": fm(z['norm2'][i]),
            "scT": parts[4], "shT": parts[3], "g2T": parts[5], "w_r": z['moe_w_router'][i],
            "b_r": np.ascontiguousarray(np.broadcast_to(z['moe_b_router'][i][None, :], (128, NE))),
            "w1": z['moe_w1'][i], "b1g": b1g, "b1l": b1l, "w2": z['moe_w2'][i], "b2": z['moe_b2'][i],
            "ident": np.eye(128, dtype=np.float32), "sel": sel.reshape(NE, NE * 128)}


NQ = 32
LCH = 128
TWO_PI = 6.283185307179586
PI = 3.141592653589793


def _tt(S, eng, out, a, b, op, reads, writes):
    S.op(eng, reads, writes, lambda e: e.tensor_tensor(out, a, b, op))


def emit_p2(nc, S, io):
    di = io.inp
    uT = di("uT", [1024, T])
    braw_re = di("braw_re", [2, 128, NQ, 128])
    braw_im = di("braw_im", [2, 128, NQ, 128])
    cT_re = di("cT_re", [2, 128, NQ, 128])
    cT_im = di("cT_im", [2, 128, NQ, 128])
    lr_row = di("lr_row", [2, 128, 4096])
    li_row = di("li_row", [2, 128, 4096])
    ldt_row = di("ldt_row", [2, 128, 4096])
    lr_p = di("lr_p", [2, 128, NQ])
    li_p = di("li_p", [2, 128, NQ])
    ldt_p = di("ldt_p", [2, 128, NQ])
    jrow_d = di("jrow", [128, LCH])
    ysT = io.out("ysT", [2, 1024, T])
    with contextlib.ExitStack() as st:
        jrow = sb(nc, st, "jrow_s", [128, LCH])
        S.dma(jrow[:], jrow_d, [], [jrow])
        BbT_re = sb(nc, st, "BbT_re", [128, NQ, 128])
        BbT_im = sb(nc, st, "BbT_im", [128, NQ, 128])
        CT_re = sb(nc, st, "CT_re", [128, NQ, 128])
        CT_imn = sb(nc, st, "CT_imn", [128, NQ, 128])
        cosT = sb(nc, st, "cosT", [128, NQ, LCH])
        sinT = sb(nc, st, "sinT", [128, NQ, LCH])
        rhoT = sb(nc, st, "rhoT", [128, NQ, LCH])
        PW = 512
        scr = [sb(nc, st, "scr%d" % i, [128, PW]) for i in range(10)]
        pp = [sb(nc, st, "pp%d" % i, [128, NQ]) for i in range(6)]
        nsl = sb(nc, st, "nsl", [128, NQ])
        hp_re = sb(nc, st, "hp_re", [128, NQ])
        hp_im = sb(nc, st, "hp_im", [128, NQ])
        tcol = sb(nc, st, "tcol", [128, 2])
        ub = sb(nc, st, "ub", [128, 8, 512])
        t1r = Ring(nc, st, "t1r", [128, 512], F32, 2)
        t2r = Ring(nc, st, "t2r", [128, 512], F32, 2)
        wre_r = Ring(nc, st, "wre", [128, 512], F32, 2)
        wim_r = Ring(nc, st, "wim", [128, 512], F32, 2)
        gre_r = Ring(nc, st, "gre", [128, 512], F32, 2)
        gim_r = Ring(nc, st, "gim", [128, 512], F32, 2)
        hre_r = Ring(nc, st, "hre", [128, 512], F32, 6)
        him_r = Ring(nc, st, "him", [128, 512], F32, 6)
        yo_r = Ring(nc, st, "yo", [128, 512], F32, 2)
        bure_r = Ring(nc, st, "bure", [128, 512], F32, 2, psum=True)
        buim_r = Ring(nc, st, "buim", [128, 512], F32, 2, psum=True)
        y_r = Ring(nc, st, "yps", [128, 512], F32, 2, psum=True)

        ki = sb(nc, st, "ki", [128, PW], mybir.dt.int32)

        def sin_of(dst, src, off, ta, tb):
            kiv = ki[:, :ta.shape[-1]]
            S.op('dve', [src], [ta], lambda e: e.tensor_scalar_add(ta, src, off))
            S.op('dve', [ta], [tb], lambda e: e.tensor_scalar_mul(tb, ta, 1.0 / TWO_PI))
            S.op('dve', [tb], [ki], lambda e: e.tensor_copy(kiv, tb))
            S.op('dve', [ki], [tb], lambda e: e.tensor_copy(tb, kiv))
            S.op('dve', [tb, ta], [ta], lambda e: e.scalar_tensor_tensor(ta, tb, -TWO_PI, ta, ALU.mult, ALU.add))
            S.op('dve', [ta], [tb], lambda e: e.tensor_scalar(tb, ta, PI, -TWO_PI, ALU.is_gt, ALU.mult))
            S.op('dve', [ta, tb], [ta], lambda e: e.tensor_tensor(ta, ta, tb, ALU.add))
            S.op('dve', [ta], [tb], lambda e: e.tensor_scalar(tb, ta, -PI, TWO_PI, ALU.is_lt, ALU.mult))
            S.op('dve', [ta, tb], [ta], lambda e: e.tensor_tensor(ta, ta, tb, ALU.add))
            S.op('dve', [ta], [ta], lambda e: e.tensor_scalar(ta, ta, -PI, PI, ALU.max, ALU.min))
            S.op('act', [ta], [dst], lambda e: e.activation(dst, ta, AF.Sin))

        for d in range(2):
            for pc in range(4096 // PW):
                sl = slice(pc * PW, (pc + 1) * PW)
                lr, li, ldt, a, th, rho, cs, sn, x1, x2 = [t[:] for t in scr]
                S.dma(lr, lr_row[d, :, sl], [], [scr[0]])
                S.dma(li, li_row[d, :, sl], [], [scr[1]])
                S.dma(ldt, ldt_row[d, :, sl], [], [scr[2]])
                S.op('act', [scr[2]], [scr[2]], lambda e: e.activation(ldt, ldt, AF.Exp))
                _tt(S, 'dve', a, lr, ldt, ALU.mult, [scr[0], scr[2]], [scr[3]])
                _tt(S, 'dve', th, li, ldt, ALU.mult, [scr[1], scr[2]], [scr[4]])
                S.op('act', [scr[3]], [scr[5]], lambda e: e.activation(rho, a, AF.Exp))
                sin_of(sn, th, 0.0, x1, x2)
                sin_of(cs, th, PI / 2, x1, x2)
                _tt(S, 'dve', cs, cs, rho, ALU.mult, [scr[6], scr[5]], [scr[6]])
                _tt(S, 'dve', sn, sn, rho, ALU.mult, [scr[7], scr[5]], [scr[7]])
                S.op('dve', [scr[6]], [scr[6]], lambda e: e.tensor_scalar_add(cs, cs, -1.0))
                _tt(S, 'dve', x1, lr, lr, ALU.mult, [scr[0]], [scr[8]])
                _tt(S, 'dve', x2, li, li, ALU.mult, [scr[1]], [scr[9]])
                _tt(S, 'dve', x1, x1, x2, ALU.add, [scr[8], scr[9]], [scr[8]])
                S.op('dve', [scr[8]], [scr[8]], lambda e: e.reciprocal(x1, x1))
                _tt(S, 'dve', a, cs, lr, ALU.mult, [scr[6], scr[0]], [scr[3]])
                _tt(S, 'dve', x2, sn, li, ALU.mult, [scr[7], scr[1]], [scr[9]])
                _tt(S, 'dve', a, a, x2, ALU.add, [scr[3], scr[9]], [scr[3]])
                _tt(S, 'dve', a, a, x1, ALU.mult, [scr[3], scr[8]], [scr[3]])
                _tt(S, 'dve', th, sn, lr, ALU.mult, [scr[7], scr[0]], [scr[4]])
                _tt(S, 'dve', x2, cs, li, ALU.mult, [scr[6], scr[1]], [scr[9]])
                _tt(S, 'dve', th, th, x2, ALU.subtract, [scr[4], scr[9]], [scr[4]])
                _tt(S, 'dve', th, th, x1, ALU.mult, [scr[4], scr[8]], [scr[4]])
                qs = slice(pc * (PW // 128), (pc + 1) * (PW // 128))
                S.dma(lr.rearrange("p (q s) -> p q s", s=128), braw_re[d, :, qs, :], [], [scr[0]])
                S.dma(li.rearrange("p (q s) -> p q s", s=128), braw_im[d, :, qs, :], [], [scr[1]])
                bre_v = BbT_re[:, qs, :].rearrange("p q s -> p (q s)")
                bim_v = BbT_im[:, qs, :].rearrange("p q s -> p (q s)")
                _tt(S, 'dve', x1, lr, a, ALU.mult, [scr[0], scr[3]], [scr[8]])
                _tt(S, 'dve', x2, li, th, ALU.mult, [scr[1], scr[4]], [scr[9]])
                _tt(S, 'dve', bre_v, x1, x2, ALU.subtract, [scr[8], scr[9]], [BbT_re])
                _tt(S, 'dve', x1, lr, th, ALU.mult, [scr[0], scr[4]], [scr[8]])
                _tt(S, 'dve', x2, li, a, ALU.mult, [scr[1], scr[3]], [scr[9]])
                _tt(S, 'dve', bim_v, x1, x2, ALU.add, [scr[8], scr[9]], [BbT_im])
            plr, pli, pldt, prho, pth, ptmp = [t[:] for t in pp]
            S.dma(plr, lr_p[d], [], [pp[0]])
            S.dma(pli, li_p[d], [], [pp[1]])
            S.dma(pldt, ldt_p[d], [], [pp[2]])
            S.op('act', [pp[2]], [pp[2]], lambda e: e.activation(pldt, pldt, AF.Exp))
            _tt(S, 'dve', ptmp, plr, pldt, ALU.mult, [pp[0], pp[2]], [pp[5]])
            S.op('act', [pp[5]], [pp[3]], lambda e: e.activation(prho, ptmp, AF.Exp))
            _tt(S, 'dve', pth, pli, pldt, ALU.mult, [pp[1], pp[2]], [pp[4]])
            for q in range(NQ):
                S.op('dve', [jrow, pp[4]], [cosT], lambda e, q=q: e.tensor_scalar(cosT[:, q, :], jrow[:], pth[:, q:q + 1], None, ALU.mult))
                S.op('dve', [jrow, pp[3]], [rhoT], lambda e, q=q: e.tensor_scalar(rhoT[:, q, :], jrow[:], 0.0, pp[3][:, q:q + 1], ALU.mult, ALU.add))
            cflat = cosT[:].rearrange("p q j -> p (q j)")
            sflat = sinT[:].rearrange("p q j -> p (q j)")
            for pc in range(4096 // PW):
                sl = slice(pc * PW, (pc + 1) * PW)
                x1 = scr[8][:]
                x2 = scr[9][:]
                sin_of(sflat[:, sl], cflat[:, sl], 0.0, x1, x2)
                sin_of(cflat[:, sl], cflat[:, sl], PI / 2, x1, x2)
            S.op('dve', [sinT], [nsl], lambda e: e.tensor_scalar_mul(nsl[:], sinT[:, :, LCH - 1], -1.0))
            S.dma(CT_re[:], cT_re[d], [], [CT_re])
            S.dma(CT_imn[:], cT_im[d], [], [CT_imn])
            S.op('pool', [CT_imn], [CT_imn], lambda e: e.tensor_scalar_mul(CT_imn[:], CT_imn[:], -1.0))
            S.op('pool', [], [hp_re], lambda e: e.memset(hp_re[:], 0.0))
            S.op('pool', [], [hp_im], lambda e: e.memset(hp_im[:], 0.0))
            for (p0, n, _c) in TOK_BLOCKS:
                t0 = p0 if d == 0 else int(ORDER_B[p0 + n - 1])
                nch = n // LCH
                S.dma(ub[:, :, :n], uT[:, t0:t0 + n].rearrange("(k p) t -> p k t", p=128), [], [ub])
                usl = (lambda k: ub[:, k, :n]) if d == 0 else (lambda k: ub[:, k, n - 1::-1])
                hres, hims = [], []
                for q in range(NQ):
                    pre = bure_r.get()
                    pim = buim_r.get()
                    S.op('pe', [BbT_re, ub], [pre], lambda e, q=q, pre=pre, usl=usl: e.matmul(pre[:, :n], BbT_re[:, q, :], usl(q // 4), start=True, stop=True))
                    S.op('pe', [BbT_im, ub], [pim], lambda e, q=q, pim=pim, usl=usl: e.matmul(pim[:, :n], BbT_im[:, q, :], usl(q // 4), start=True, stop=True))
                    cb = cosT[:, q:q + 1, :].to_broadcast([128, nch, LCH])
                    sbb = sinT[:, q:q + 1, :].to_broadcast([128, nch, LCH])
                    v3 = lambda t: t[:, :n].rearrange("p (c j) -> p c j", j=LCH)
                    t1 = t1r.get()
                    t2 = t2r.get()
                    wre = wre_r.get()
                    wim = wim_r.get()
                    _tt(S, 'dve', v3(t1), v3(pre), cb, ALU.mult, [pre, cosT], [t1])
                    _tt(S, 'dve', v3(t2), v3(pim), sbb, ALU.mult, [pim, sinT], [t2])
                    _tt(S, 'pool', wre[:, :n], t1[:, :n], t2[:, :n], ALU.add, [t1, t2], [wre])
                    t1 = t1r.get()
                    t2 = t2r.get()
                    _tt(S, 'dve', v3(t1), v3(pim), cb, ALU.mult, [pim, cosT], [t1])
                    _tt(S, 'dve', v3(t2), v3(pre), sbb, ALU.mult, [pre, sinT], [t2])
                    _tt(S, 'pool', wim[:, :n], t1[:, :n], t2[:, :n], ALU.subtract, [t1, t2], [wim])
                    gre = gre_r.get()
                    gim = gim_r.get()
                    for ch in range(nch):
                        cs_ = slice(ch * LCH, (ch + 1) * LCH)
                        last = (ch + 1) * LCH - 1
                        S.op('dve', [wre, rhoT, hp_re], [gre],
                             lambda e, q=q, cs_=cs_, gre=gre, wre=wre: e.tensor_tensor_scan(
                                 gre[:, cs_], rhoT[:, q, :], wre[:, cs_], hp_re[:, q:q + 1], ALU.mult, ALU.add))
                        S.op('dve', [wim, rhoT, hp_im], [gim],
                             lambda e, q=q, cs_=cs_, gim=gim, wim=wim: e.tensor_tensor_scan(
                                 gim[:, cs_], rhoT[:, q, :], wim[:, cs_], hp_im[:, q:q + 1], ALU.mult, ALU.add))
                        S.op('dve', [gre, cosT], [tcol],
                             lambda e, q=q, last=last, gre=gre: e.tensor_tensor(tcol[:, 0:1], gre[:, last:last + 1], cosT[:, q, LCH - 1:LCH], ALU.mult))
                        S.op('dve', [gim, cosT], [tcol],
                             lambda e, q=q, last=last, gim=gim: e.tensor_tensor(tcol[:, 1:2], gim[:, last:last + 1], cosT[:, q, LCH - 1:LCH], ALU.mult))
                        S.op('dve', [gim, nsl, tcol], [hp_re],
                             lambda e, q=q, last=last, gim=gim: e.scalar_tensor_tensor(
                                 hp_re[:, q:q + 1], gim[:, last:last + 1], nsl[:, q:q + 1], tcol[:, 0:1], ALU.mult, ALU.add))
                        S.op('dve', [gre, sinT, tcol], [hp_im],
                             lambda e, q=q, last=last, gre=gre: e.scalar_tensor_tensor(
                                 hp_im[:, q:q + 1], gre[:, last:last + 1], sinT[:, q, LCH - 1:LCH], tcol[:, 1:2], ALU.mult, ALU.add))
                    hre = hre_r.get()
                    him = him_r.get()
                    t1 = t1r.get()
                    t2 = t2r.get()
                    _tt(S, 'pool', v3(t1), v3(gre), cb, ALU.mult, [gre, cosT], [t1])
                    _tt(S, 'pool', v3(t2), v3(gim), sbb, ALU.mult, [gim, sinT], [t2])
                    _tt(S, 'pool', hre[:, :n], t1[:, :n], t2[:, :n], ALU.subtract, [t1, t2], [hre])
                    t1 = t1r.get()
                    t2 = t2r.get()
                    _tt(S, 'pool', v3(t1), v3(gre), sbb, ALU.mult, [gre, sinT], [t1])
                    _tt(S, 'pool', v3(t2), v3(gim), cb, ALU.mult, [gim, cosT], [t2])
                    _tt(S, 'pool', him[:, :n], t1[:, :n], t2[:, :n], ALU.add, [t1, t2], [him])
                    hres.append(hre)
                    hims.append(him)
                    if q % 4 == 3:
                        m = q // 4
                        yps = y_r.get()

                        def mmy(e, yps=yps, m=m, hres=list(hres), hims=list(hims)):
                            ins = None
                            for i4 in range(4):
                                qq = m * 4 + i4
                                e.matmul(yps[:, :n], CT_re[:, qq, :], hres[i4][:, :n], start=(i4 == 0), stop=False)
                                ins = e.matmul(yps[:, :n], CT_imn[:, qq, :], hims[i4][:, :n], start=False, stop=(i4 == 3))
                            return ins
                        S.op('pe', [CT_re, CT_imn] + hres + hims, [yps], mmy)
                        yo = yo_r.get()
                        S.op('act', [yps], [yo], lambda e, yo=yo, yps=yps, d=d: e.copy(yo[:, :n] if d == 0 else yo[:, n - 1::-1], yps[:, :n]))
                        S.dma(ysT[d, m * 128:(m + 1) * 128, t0:t0 + n], yo[:, :n], [yo], [])
                        hres, hims = [], []
        S.barrier()


ORDER_B = np.concatenate([np.arange(LCTX - 1, -1, -1), np.arange(T - 1, LCTX - 1, -1)])


def p2_inputs(z, j, uT):
    b_re, b_im = z['s5_b_re'][j], z['s5_b_im'][j]
    c_re, c_im = z['s5_c_re'][j], z['s5_c_im'][j]
    braw = np.zeros((2, 2, 128, NQ, 128), np.float32)
    craw = np.zeros((2, 2, 128, NQ, 128), np.float32)
    for d in range(2):
        for q in range(NQ):
            for s2 in range(2):
                g = 2 * q + s2
                cp0 = (g % 8) * 16
                braw[0, d, cp0:cp0 + 16, q, s2 * 64:(s2 + 1) * 64] = b_re[d, g].T
                braw[1, d, cp0:cp0 + 16, q, s2 * 64:(s2 + 1) * 64] = b_im[d, g].T
                craw[0, d, s2 * 64:(s2 + 1) * 64, q, cp0:cp0 + 16] = c_re[d, g].T
                craw[1, d, s2 * 64:(s2 + 1) * 64, q, cp0:cp0 + 16] = c_im[d, g].T
    lam_re, lam_im, log_dt = z['s5_lam_re'][j], z['s5_lam_im'][j], z['s5_log_dt'][j]
    ldt_full = np.repeat(log_dt, 64, axis=1)
    row = lambda v: np.ascontiguousarray(np.broadcast_to(v.reshape(2, 1, 4096), (2, 128, 4096)))
    pl = lambda v: np.ascontiguousarray(v.reshape(2, NQ, 128).transpose(0, 2, 1))
    uT2 = np.stack([uT, uT[:, ORDER_B]], 0)
    return {"uT2": np.ascontiguousarray(uT2), "braw_re": braw[0], "braw_im": braw[1], "cT_re": craw[0], "cT_im": craw[1],
            "lr_row": row(lam_re), "li_row": row(lam_im), "ldt_row": row(ldt_full),
            "lr_p": pl(lam_re), "li_p": pl(lam_im), "ldt_p": pl(ldt_full),
            "jrow": np.ascontiguousarray(np.broadcast_to(np.arange(1, LCH + 1, dtype=np.float32)[None, :], (128, LCH)))}


def emit_p2b(nc, S, io):
    di = io.inp
    uT = di("uT", [1024, T])
    yfT = di("yfT", [1024, T])
    ybT = di("ybT", [1024, T])
    dsk = di("dsk", [128, 8])
    glu_w = di("glu_w", [1024, 1024])
    glu_b = di("glu_b", [128, 8])
    yaT = io.out("yaT", [1024, T])
    KH = 8
    with contextlib.ExitStack() as st:
        d_sb = sb(nc, st, "d_sb", [128, KH])
        S.dma(d_sb[:], dsk, [], [d_sb])
        gb_sb = sb(nc, st, "gb_sb", [128, KH])
        S.dma(gb_sb[:], glu_b, [], [gb_sb])
        u_sb = sb(nc, st, "u_sb", [128, KH, 512])
        yf_sb = sb(nc, st, "yf_sb", [128, KH, 512])
        yb_sb = sb(nc, st, "yb_sb", [128, KH, 512])
        g_sb = sb(nc, st, "g_sb", [128, KH, 512])
        gT = sb(nc, st, "gT", [128, KH, 512], BF16)
        t_r = Ring(nc, st, "tt", [128, 512], F32, 3)
        o_r = Ring(nc, st, "oo", [128, 512], F32, 3)
        R = {'w32': Ring(nc, st, "w32", [128, 4096], F32, 2), 'wbf': Ring(nc, st, "wbf", [128, 4096], BF16, 2),
             'ps': Ring(nc, st, "ps", [128, 512], F32, 4, psum=True)}
        v = lambda a: a.rearrange("(k p) t -> p k t", p=128)
        for t0 in range(0, T, 512):
            n = min(512, T - t0)
            S.dma(u_sb[:, :, :n], v(uT)[:, :, t0:t0 + n], [], [u_sb])
            S.dma(yf_sb[:, :, :n], v(yfT)[:, :, t0:t0 + n], [], [yf_sb])
            S.dma(yb_sb[:, :, :n], v(ybT)[:, :, t0:t0 + n], [], [yb_sb])
            for k in range(KH):
                y = t_r.get()
                x3 = t_r.get()
                S.op('dve', [u_sb, yf_sb, d_sb], [y],
                     lambda e, k=k, y=y: e.scalar_tensor_tensor(y[:, :n], u_sb[:, k, :n], d_sb[:, k:k + 1], yf_sb[:, k, :n], ALU.mult, ALU.add))
                S.op('dve', [y, yb_sb], [y], lambda e, k=k, y=y: e.tensor_tensor(y[:, :n], y[:, :n], yb_sb[:, k, :n], ALU.add))
                S.op('act', [y], [x3], lambda e, y=y, x3=x3: e.activation(x3[:, :n], y[:, :n], AF.Square))
                S.op('dve', [x3], [x3], lambda e, x3=x3: e.tensor_scalar(x3[:, :n], x3[:, :n], 0.044715, 1.0, ALU.mult, ALU.add))
                S.op('dve', [x3, y], [x3], lambda e, y=y, x3=x3: e.tensor_tensor(x3[:, :n], x3[:, :n], y[:, :n], ALU.mult))
                S.op('act', [x3], [x3], lambda e, x3=x3: e.activation(x3[:, :n], x3[:, :n], AF.Sigmoid, scale=1.5957691216057308))
                S.op('dve', [x3, y], [(g_sb, k)], lambda e, k=k, y=y, x3=x3: e.tensor_tensor(g_sb[:, k, :n], x3[:, :n], y[:, :n], ALU.mult))
                S.op('pool', [(g_sb, k)], [(gT, k)], lambda e, k=k: e.tensor_copy(gT[:, k, :n], g_sb[:, k, :n]))

            def evac(m, ps, n=n, t0=t0):
                o = o_r.get()
                S.op('act', [ps, gb_sb], [o], lambda e: e.activation(o[:, :n], ps[:, :n], AF.Sigmoid, bias=gb_sb[:, m:m + 1], scale=1.0))
                S.op('dve', [o, (g_sb, m)], [o], lambda e: e.tensor_tensor(o[:, :n], o[:, :n], g_sb[:, m, :n], ALU.mult))
                S.dma(yaT[m * 128:(m + 1) * 128, t0:t0 + n], o[:, :n], [o], [])
            emit_linear_block_flat(S, nc, R, gT, n, KH, glu_w, 1024, evac)
        S.barrier()


INV_B = np.argsort(ORDER_B)


GRID = 64
NWIN = 640
NEG = -30000.0


def na_ws(m):
    return int(np.clip(2 * m - 4, 0, 54))


def na_pat(m):
    return {0: 0, 1: 1, 30: 3, 31: 4}.get(m, 2)


def emit_p3(nc, S, io):
    di = io.inp
    qT = di("qT", [1024, T])
    kT = di("kT", [1024, T])
    v_tm = di("v_tm", [T, 1024])
    bias_d = di("bias", [8, 5, 128, NWIN])
    ident_d = di("ident", [128, 128])
    ybT = io.out("ybT", [1024, T])
    NB = T // 128
    scale = 128 ** -0.5
    with contextlib.ExitStack() as st:
        idf = sb(nc, st, "idf", [128, 128])
        S.dma(idf[:], ident_d, [], [idf])
        idb = sb(nc, st, "idb", [128, 128], BF16)
        S.op('dve', [idf], [idb], lambda e: e.tensor_copy(idb[:], idf[:]))
        stage = sb(nc, st, "stage", [128, T])
        q_r = Ring(nc, st, "qb", [128, T], BF16, 2)
        k_r = Ring(nc, st, "kb", [128, T], BF16, 2)
        v_r = Ring(nc, st, "vb", [128, NB, 128], BF16, 2)
        b_r = Ring(nc, st, "bia", [128, 5, NWIN], F32, 2)
        sc_r = Ring(nc, st, "sc", [128, 896], F32, 2)
        p_r = Ring(nc, st, "pp", [128, 896], BF16, 2)
        pt_r = Ring(nc, st, "ptT", [128, 896], BF16, 2)
        o_r = Ring(nc, st, "o", [128, 128], F32, 4)
        sm_r = Ring(nc, st, "sm", [128, 4], F32, 4)
        psA = Ring(nc, st, "psA", [128, 512], F32, 2, psum=True)
        psB = Ring(nc, st, "psB", [128, 512], F32, 2, psum=True)
        psT = Ring(nc, st, "psT", [128, 1024], BF16, 2, psum=True)
        psO = Ring(nc, st, "psO", [128, 128], F32, 2, psum=True)
        for h in range(8):
            hs = slice(h * 128, (h + 1) * 128)
            qb = q_r.get()
            kb = k_r.get()
            vb = v_r.get()
            bia = b_r.get()
            S.dma(stage[:], qT[hs, :], [], [stage])
            S.op('act', [stage], [qb], lambda e, qb=qb: e.copy(qb[:], stage[:]))
            S.dma(stage[:], kT[hs, :], [], [stage])
            S.op('dve', [stage], [kb], lambda e, kb=kb: e.tensor_copy(kb[:], stage[:]))
            S.dma(stage[:].rearrange("p (b e) -> p b e", e=128), v_tm[:, hs].rearrange("(b p) e -> p b e", p=128), [], [stage])
            S.op('pool', [stage], [vb], lambda e, vb=vb: e.tensor_copy(vb[:], stage[:].rearrange("p (b e) -> p b e", e=128)))
            S.dma(bia[:], bias_d[h].rearrange("a p k -> p a k"), [], [bia])
            for blk in range(NB):
                q0 = blk * 128
                is_ctx = blk < 2
                if is_ctx:
                    nk = LCTX
                    kblocks = [0, 1]
                else:
                    m = blk - 2
                    ws = na_ws(m)
                    pat = na_pat(m)
                    nk = LCTX + NWIN
                    w0 = LCTX + ws * GRID
                    kblocks = [0, 1] + [2 + ws // 2 + i for i in range(5)]
                pa = psA.get()
                pb = psB.get()

                def mms(e, pa=pa, pb=pb, qb=qb, kb=kb, q0=q0, is_ctx=is_ctx, w0=(0 if is_ctx else w0)):
                    ins = e.matmul(pa[:, 0:256], qb[:, q0:q0 + 128], kb[:, 0:256], start=True, stop=True)
                    if not is_ctx:
                        e.matmul(pa[:, 256:512], qb[:, q0:q0 + 128], kb[:, w0:w0 + 256], start=True, stop=True)
                        ins = e.matmul(pb[:, 0:384], qb[:, q0:q0 + 128], kb[:, w0 + 256:w0 + 640], start=True, stop=True)
                    return ins
                S.op('pe', [qb, kb], [pa, pb], mms)
                sc = sc_r.get()
                S.op('act', [pa], [sc], lambda e, sc=sc, pa=pa: e.mul(sc[:, 0:256], pa[:, 0:256], scale))
                if not is_ctx:
                    S.op('dve', [pa, bia, sc], [sc],
                         lambda e, sc=sc, pa=pa, bia=bia, pat=pat: e.scalar_tensor_tensor(sc[:, 256:512], pa[:, 256:512], scale, bia[:, pat, 0:256], ALU.mult, ALU.add))
                    S.op('dve', [pb, bia, sc], [sc],
                         lambda e, sc=sc, pb=pb, bia=bia, pat=pat: e.scalar_tensor_tensor(sc[:, 512:896], pb[:, 0:384], scale, bia[:, pat, 256:640], ALU.mult, ALU.add))
                sm = sm_r.get()
                S.op('dve', [sc], [sm], lambda e, sc=sc, sm=sm, nk=nk: e.reduce_max(sm[:, 0:1], sc[:, :nk], AX.X))
                S.op('dve', [sm], [sm], lambda e, sm=sm: e.tensor_scalar_mul(sm[:, 1:2], sm[:, 0:1], -1.0))
                p = p_r.get()
                S.op('act', [sc, sm], [p, sm],
                     lambda e, sc=sc, sm=sm, p=p, nk=nk: e.activation(p[:, :nk], sc[:, :nk], AF.Exp, bias=sm[:, 1:2], scale=1.0, accum_out=sm[:, 2:3]))
                S.op('dve', [sm], [sm], lambda e, sm=sm: e.reciprocal(sm[:, 3:4], sm[:, 2:3]))
                pT = psT.get()
                nkb = len(kblocks)

                def trs(e, p=p, pT=pT, nkb=nkb):
                    ins = None
                    for i in range(nkb):
                        ins = e.transpose(pT[:, i * 128:(i + 1) * 128], p[:, i * 128:(i + 1) * 128], idb[:])
                    return ins
                S.op('pe', [p, idb], [pT], trs)
                pts = pt_r.get()
                S.op('dve', [pT], [pts], lambda e, pts=pts, pT=pT, nkb=nkb: e.tensor_copy(pts[:, :nkb * 128], pT[:, :nkb * 128]))
                po = psO.get()

                def mmo(e, pts=pts, po=po, vb=vb, kblocks=kblocks):
                    ins = None
                    for i, kbk in enumerate(kblocks):
                        ins = e.matmul(po[:], pts[:, i * 128:(i + 1) * 128], vb[:, kbk, :], start=(i == 0), stop=(i == len(kblocks) - 1))
                    return ins
                S.op('pe', [pts, vb], [po], mmo)
                o = o_r.get()
                S.op('act', [po, sm], [o], lambda e, o=o, po=po, sm=sm: e.mul(o[:], po[:], sm[:, 3:4]))
                po2 = psO.get()
                S.op('pe', [o, idf], [po2], lambda e, o=o, po2=po2: e.transpose(po2[:], o[:], idf[:]))
                o2 = o_r.get()
                S.op('dve', [po2], [o2], lambda e, o2=o2, po2=po2: e.tensor_copy(o2[:], po2[:]))
                S.dma(ybT[hs, q0:q0 + 128], o2[:], [o2], [])
        S.barrier()


def na_bias_table(rpb):
    rows = np.arange(GRID)
    row_start = np.clip(rows - 4, 0, GRID - 8)
    col = np.arange(GRID)
    col_start = np.clip(col - 8, 0, GRID - 16)
    out = np.full((8, 5, 128, NWIN), NEG, np.float32)
    for pat, m in enumerate([0, 1, 2, 30, 31]):
        ws = na_ws(m)
        for qi in range(128):
            r = 2 * m + qi // 64
            c = qi % 64
            for kr_i in range(10):
                kr = ws + kr_i
                if not (row_start[r] <= kr < row_start[r] + 8):
                    continue
                kc = np.arange(col_start[c], col_start[c] + 16)
                dr = kr - r + 7
                dc = np.clip(kc - c + 15, 0, 30)
                out[:, pat, qi, kr_i * 64 + kc] = rpb[:, dr, dc]
    return out


def emit_p5(nc, S, io):
    di = io.inp
    qT = di("qT", [1024, T])
    kT = di("kT", [256, T])
    v_tm = di("v_tm", [T, 256])
    gq_d = di("gq", [128, 1])
    gk_d = di("gk", [128, 1])
    ropeC_d = di("ropeC", [128, SEQ])
    ropeS_d = di("ropeS", [128, SEQ])
    perm_d = di("perm", [128, 128])
    ident_d = di("ident", [128, 128])
    ydT = io.out("ydT", [1024, T])
    NB = T // 128
    scale = 128 ** -0.5
    with contextlib.ExitStack() as st:
        idf = sb(nc, st, "idf", [128, 128])
        S.dma(idf[:], ident_d, [], [idf])
        idb = sb(nc, st, "idb", [128, 128], BF16)
        S.op('dve', [idf], [idb], lambda e: e.tensor_copy(idb[:], idf[:]))
        perm = sb(nc, st, "perm_s", [128, 128])
        S.dma(perm[:], perm_d, [], [perm])
        ones = sb(nc, st, "ones", [128, 128])
        S.op('pool', [], [ones], lambda e: e.memset(ones[:], 1.0))
        gq = sb(nc, st, "gq_s", [128, 1])
        gk = sb(nc, st, "gk_s", [128, 1])
        S.dma(gq[:], gq_d, [], [gq])
        S.dma(gk[:], gk_d, [], [gk])
        S.op('dve', [gq], [gq], lambda e: e.tensor_scalar_mul(gq[:], gq[:], scale))
        ropeC = sb(nc, st, "ropeC_s", [128, SEQ])
        ropeS = sb(nc, st, "ropeS_s", [128, SEQ])
        S.dma(ropeC[:], ropeC_d, [], [ropeC])
        S.dma(ropeS[:], ropeS_d, [], [ropeS])
        stage = sb(nc, st, "stage", [128, T])
        q_r = Ring(nc, st, "qb", [128, T], BF16, 2)
        kb = sb(nc, st, "kb", [128, T], BF16)
        vb = sb(nc, st, "vb", [128, NB, 128], BF16)
        t_r = Ring(nc, st, "tt", [128, 512], F32, 4)
        rs_r = Ring(nc, st, "rs", [128, 512], F32, 2)
        sc_r = Ring(nc, st, "sc", [128, T], F32, 2)
        p_r = Ring(nc, st, "pp", [128, T], BF16, 2)
        pt_r = Ring(nc, st, "ptT", [128, 1024], BF16, 3)
        o_r = Ring(nc, st, "o", [128, 128], F32, 4)
        sm_r = Ring(nc, st, "sm", [128, 4], F32, 4)
        psS = Ring(nc, st, "psS", [128, 512], F32, 3, psum=True)
        psT = Ring(nc, st, "psT", [128, 1024], BF16, 2, psum=True)
        psO = Ring(nc, st, "psO", [128, 128], F32, 2, psum=True)
        psN = Ring(nc, st, "psN", [128, 512], F32, 1, psum=True)

        def prep(src_rows, g, dst):
            S.dma(stage[:], src_rows, [], [stage])
            for (t0, n, col) in TOK_BLOCKS:
                sqc = t_r.get()
                S.op('act', [stage], [sqc], lambda e, sqc=sqc: e.activation(sqc[:, :n], stage[:, t0:t0 + n], AF.Square))
                pn = psN.get()
                S.op('pe', [sqc, ones], [pn], lambda e, sqc=sqc, pn=pn: e.matmul(pn[:, :n], ones[:], sqc[:, :n], start=True, stop=True))
                rs = rs_r.get()
                S.op('dve', [pn], [rs], lambda e, rs=rs, pn=pn: e.tensor_scalar(rs[:, :n], pn[:, :n], 1.0 / 128, EPS, ALU.mult, ALU.add))
                S.op('act', [rs], [rs], lambda e, rs=rs: e.activation(rs[:, :n], rs[:, :n], AF.Sqrt))
                S.op('dve', [rs], [rs], lambda e, rs=rs: e.reciprocal(rs[:, :n], rs[:, :n]))
                qn = t_r.get()
                S.op('dve', [stage, g, rs], [qn],
                     lambda e, qn=qn, rs=rs: e.scalar_tensor_tensor(qn[:, :n], stage[:, t0:t0 + n], g[:, 0:1], rs[:, :n], ALU.mult, ALU.mult))
                if col == 1:
                    S.op('act', [qn], [dst], lambda e, qn=qn: e.copy(dst[:, t0:t0 + n], qn[:, :n]))
                else:
                    p0 = t0 - LCTX
                    pn2 = psN.get()
                    S.op('pe', [qn, perm], [pn2], lambda e, qn=qn, pn2=pn2: e.matmul(pn2[:, :n], perm[:], qn[:, :n], start=True, stop=True))
                    a1 = t_r.get()
                    a2 = t_r.get()
                    S.op('dve', [qn, ropeC], [a1], lambda e, qn=qn, a1=a1: e.tensor_tensor(a1[:, :n], qn[:, :n], ropeC[:, p0:p0 + n], ALU.mult))
                    S.op('dve', [pn2, ropeS], [a2], lambda e, pn2=pn2, a2=a2: e.tensor_tensor(a2[:, :n], pn2[:, :n], ropeS[:, p0:p0 + n], ALU.mult))
                    S.op('pool', [a1, a2], [dst], lambda e, a1=a1, a2=a2: e.tensor_tensor(dst[:, t0:t0 + n], a1[:, :n], a2[:, :n], ALU.add))

        for kvh in range(2):
            prep(kT[kvh * 128:(kvh + 1) * 128, :], gk, kb)
            S.dma(stage[:].rearrange("p (b e) -> p b e", e=128),
                  v_tm[:, kvh * 128:(kvh + 1) * 128].rearrange("(b p) e -> p b e", p=128), [], [stage])
            S.op('pool', [stage], [vb], lambda e: e.tensor_copy(vb[:], stage[:].rearrange("p (b e) -> p b e", e=128)))
            for hh in range(4):
                h = kvh * 4 + hh
                hs = slice(h * 128, (h + 1) * 128)
                qb = q_r.get()
                prep(qT[hs, :], gq, qb)
                for blk in range(NB):
                    q0 = blk * 128
                    nk = LCTX if blk < 2 else T
                    nkb = nk // 128
                    sc = sc_r.get()
                    for c0 in range(0, nk, 512):
                        cw = min(512, nk - c0)
                        ps = psS.get()
                        S.op('pe', [qb, kb], [ps],
                             lambda e, ps=ps, qb=qb, q0=q0, c0=c0, cw=cw: e.matmul(ps[:, :cw], qb[:, q0:q0 + 128], kb[:, c0:c0 + cw], start=True, stop=True))
                        if (c0 // 512) % 2 == 0:
                            S.op('act', [ps], [sc], lambda e, ps=ps, sc=sc, c0=c0, cw=cw: e.copy(sc[:, c0:c0 + cw], ps[:, :cw]))
                        else:
                            S.op('dve', [ps], [sc], lambda e, ps=ps, sc=sc, c0=c0, cw=cw: e.tensor_copy(sc[:, c0:c0 + cw], ps[:, :cw]))
                    sm = sm_r.get()
                    S.op('dve', [sc], [sm], lambda e, sc=sc, sm=sm, nk=nk: e.reduce_max(sm[:, 0:1], sc[:, :nk], AX.X))
                    S.op('dve', [sm], [sm], lambda e, sm=sm: e.tensor_scalar_mul(sm[:, 1:2], sm[:, 0:1], -1.0))
                    p = p_r.get()
                    S.op('act', [sc, sm], [p, sm],
                         lambda e, sc=sc, sm=sm, p=p, nk=nk: e.activation(p[:, :nk], sc[:, :nk], AF.Exp, bias=sm[:, 1:2], scale=1.0, accum_out=sm[:, 2:3]))
                    S.op('dve', [sm], [sm], lambda e, sm=sm: e.reciprocal(sm[:, 3:4], sm[:, 2:3]))
                    po = psO.get()
                    for g0 in range(0, nkb, 8):
                        gn = min(8, nkb - g0)
                        pT = psT.get()

                        def trs(e, p=p, pT=pT, g0=g0, gn=gn):
                            ins = None
                            for i in range(gn):
                                ins = e.transpose(pT[:, i * 128:(i + 1) * 128], p[:, (g0 + i) * 128:(g0 + i + 1) * 128], idb[:])
                            return ins
                        S.op('pe', [p, idb], [pT], trs)
                        pts = pt_r.get()
                        if (g0 // 8) % 2 == 0:
                            S.op('dve', [pT], [pts], lambda e, pts=pts, pT=pT, gn=gn: e.tensor_copy(pts[:, :gn * 128], pT[:, :gn * 128]))
                        else:
                            S.op('pool', [pT], [pts], lambda e, pts=pts, pT=pT, gn=gn: e.tensor_copy(pts[:, :gn * 128], pT[:, :gn * 128])) if False else \
                                S.op('act', [pT], [pts], lambda e, pts=pts, pT=pT, gn=gn: e.copy(pts[:, :gn * 128], pT[:, :gn * 128]))

                        def mmo(e, pts=pts, po=po, g0=g0, gn=gn, nkb=nkb):
                            ins = None
                            for i in range(gn):
                                ins = e.matmul(po[:], pts[:, i * 128:(i + 1) * 128], vb[:, g0 + i, :], start=(g0 + i == 0), stop=(g0 + i == nkb - 1))
                            return ins
                        S.op('pe', [pts, vb], [po], mmo)
                    o = o_r.get()
                    S.op('act', [po, sm], [o], lambda e, o=o, po=po, sm=sm: e.mul(o[:], po[:], sm[:, 3:4]))
                    po2 = psO.get()
                    S.op('pe', [o, idf], [po2], lambda e, o=o, po2=po2: e.transpose(po2[:], o[:], idf[:]))
                    o2 = o_r.get()
                    S.op('dve', [po2], [o2], lambda e, o2=o2, po2=po2: e.tensor_copy(o2[:], po2[:]))
                    S.dma(ydT[hs, q0:q0 + 128], o2[:], [o2], [])
        S.barrier()


def rope_tables():
    pos = np.arange(SEQ)
    rowp, colp = pos // GRID, pos % GRID
    inv = 10000.0 ** (-np.arange(0, 64, 2, dtype=np.float32) / 64.0)
    C = np.zeros((128, SEQ), np.float32)
    Sg = np.zeros((128, SEQ), np.float32)
    perm = np.zeros((128, 128), np.float32)
    for e in range(128):
        p = rowp if e < 64 else colp
        i = (e % 64) % 32
        ang = p.astype(np.float32) * inv[i]
        C[e] = np.cos(ang)
        first = (e % 64) < 32
        Sg[e] = -np.sin(ang) if first else np.sin(ang)
        partner = e + 32 if first else e - 32
        perm[partner, e] = 1.0
    return C, Sg, perm


RK_IN = 3456
SEGS = [(0, LCTX), (LCTX, T)]


def emit_p4a(nc, S, io):
    di = io.inp
    zT = di("zT", [RK_IN, T])
    mu0 = di("mu0", [128, 27])
    mu1 = di("mu1", [128, 27])
    g_up = di("g_up", [128, 1024])
    w_up = di("w_up", [128, 1024])
    a_up = di("a_up", [128, 1024])
    w0 = di("w0", [128, 2, 8])
    a0 = di("a0", [128, 2, 8])
    k_k = di("k_k", [128, 2, 8])
    k_a = di("k_a", [128, 2, 8])
    r_k = di("r_k", [128, 8])
    bones_d = di("bones", [128, 128])
    ident_d = di("ident", [128, 128])
    vT_o = io.out("vT", [1024, T])
    gateT_o = io.out("gateT", [1024, T])
    bonT_o = io.out("bonT", [1024, T])
    X5 = io.out("X5", [T, 2, 2, 5, 8, 64])
    with contextlib.ExitStack() as st:

        def ld(name, src, shape):
            t = sb(nc, st, name, shape)
            S.dma(t[:], src, [], [t])
            return t
        mu0s, mu1s = ld("mu0s", mu0, [128, 27]), ld("mu1s", mu1, [128, 27])
        gups, wups, aups = ld("gups", g_up, [128, 1024]), ld("wups", w_up, [128, 1024]), ld("aups", a_up, [128, 1024])
        w0s, a0s, kks, kas = ld("w0s", w0, [128, 2, 8]), ld("a0s", a0, [128, 2, 8]), ld("kks", k_k, [128, 2, 8]), ld("kas", k_a, [128, 2, 8])
        rks = ld("rks", r_k, [128, 8])
        bones = ld("bones_s", bones_d, [128, 128])
        c0s = sb(nc, st, "c0s", [128, 27])
        S.op('dve', [mu0s, mu1s], [c0s], lambda e: e.tensor_tensor(c0s[:], mu0s[:], mu1s[:], ALU.add))
        S.op('dve', [c0s], [c0s], lambda e: e.tensor_scalar(c0s[:], c0s[:], -1.0, 1.0, ALU.mult, ALU.add))
        nw0 = sb(nc, st, "nw0", [128, 2, 8])
        S.op('dve', [w0s], [nw0], lambda e: e.tensor_scalar_mul(nw0[:], w0s[:], -1.0))
        raw = sb(nc, st, "raw", [128, T])

        def shifted(chunk, dst):
            S.dma(raw[:], zT[chunk * 128:(chunk + 1) * 128, :], [], [raw])
            S.op('dve', [raw, c0s], [dst], lambda e: e.tensor_scalar(dst[:], raw[:], c0s[:, chunk:chunk + 1], None, ALU.mult))
            for (s0, s1) in SEGS:
                S.op('dve', [raw, mu0s, dst], [dst],
                     lambda e, s0=s0, s1=s1: e.scalar_tensor_tensor(dst[:, s0 + 1:s1], raw[:, s0:s1 - 1], mu0s[:, chunk:chunk + 1], dst[:, s0 + 1:s1], ALU.mult, ALU.add))
                S.op('dve', [raw, mu1s, dst], [dst],
                     lambda e, s0=s0, s1=s1: e.scalar_tensor_tensor(dst[:, s0:s1 - 1], raw[:, s0 + 1:s1], mu1s[:, chunk:chunk + 1], dst[:, s0:s1 - 1], ALU.mult, ALU.add))
        sgT = sb(nc, st, "sgT", [128, T])
        twT = sb(nc, st, "twT", [128, T])
        alT = sb(nc, st, "alT", [128, T])
        shifted(24, sgT)
        S.op('act', [sgT], [sgT], lambda e: e.activation(sgT[:], sgT[:], AF.Sigmoid))
        shifted(25, twT)
        S.op('act', [twT], [twT], lambda e: e.activation(twT[:], twT[:], AF.Tanh))
        shifted(26, alT)
        rS = sb(nc, st, "rS", [128, T])
        kS = sb(nc, st, "kS", [128, T])
        vS = sb(nc, st, "vS", [128, T])
        tr = Ring(nc, st, "tr", [128, 512], F32, 8)
        orr = Ring(nc, st, "orr", [128, 512], F32, 6)
        psr = Ring(nc, st, "ps", [128, 512], F32, 5, psum=True)
        pst = Ring(nc, st, "pst", [128, 128], F32, 2, psum=True)
        xo_r = Ring(nc, st, "xo", [128, 128], F32, 4)
        idf = ld("idf", ident_d, [128, 128])
        psb = Ring(nc, st, "psb", [128, 512], F32, 1, psum=True)

        SLOT = {0: [(0, 4), (1, 4)], 4: [(0, 0)], 5: [(0, 3)], 6: [(0, 1)], 7: [(0, 2)],
                8: [(1, 0)], 9: [(1, 3)], 10: [(1, 1)], 11: [(1, 2)]}
        FM = {1: vT_o, 2: gateT_o, 3: bonT_o}
        xcnt = [0]

        def store(idx, c, t0, n, tile):
            if idx in FM:
                S.dma(FM[idx][c * 128:(c + 1) * 128, t0:t0 + n], tile[:, :n], [tile], [])
                return
            for tb in range(n // 128):
                pt = pst.get()
                S.op('pe', [tile, idf], [pt], lambda e, pt=pt, tb=tb: e.transpose(pt[:], tile[:, tb * 128:(tb + 1) * 128], idf[:]))
                xo = xo_r.get()
                if xcnt[0] % 2 == 0:
                    S.op('act', [pt], [xo], lambda e, pt=pt, xo=xo: e.copy(xo[:], pt[:]))
                else:
                    S.op('pool', [pt], [xo], lambda e: None) if False else S.op('dve', [pt], [xo], lambda e, pt=pt, xo=xo: e.tensor_copy(xo[:], pt[:]))
                xcnt[0] += 1
                for (d, s_) in SLOT[idx]:
                    S.dma(X5[t0 + tb * 128:t0 + (tb + 1) * 128, :, d, s_, c, :], xo[:].rearrange("p (j k) -> p j k", j=2), [xo], [])
        for c in range(8):
            shifted(c, rS)
            shifted(8 + c, kS)
            shifted(16 + c, vS)
            cs = slice(c * 128, (c + 1) * 128)
            for t0 in range(0, T, 512):
                n = min(512, T - t0)
                ts_ = slice(t0, t0 + n)
                o = orr.get()
                S.op('act', [rS], [o], lambda e, o=o: e.copy(o[:, :n], rS[:, ts_]))
                store(0, c, t0, n, o)
                o = orr.get()
                S.op('act', [vS], [o], lambda e, o=o: e.copy(o[:, :n], vS[:, ts_]))
                store(1, c, t0, n, o)
                ps = psr.get()
                S.op('pe', [gups, sgT], [ps], lambda e, ps=ps: e.matmul(ps[:, :n], gups[:, cs], sgT[:, ts_], start=True, stop=True))
                o = orr.get()
                S.op('act', [ps], [o], lambda e, o=o, ps=ps: e.copy(o[:, :n], ps[:, :n]))
                store(2, c, t0, n, o)
                pbon = psb.get()
                for d in range(2):
                    ds = slice(d * 64, (d + 1) * 64)
                    ps = psr.get()
                    S.op('pe', [wups, twT], [ps], lambda e, ps=ps: e.matmul(ps[:, :n], wups[ds, cs], twT[ds, ts_], start=True, stop=True))
                    y = tr.get()
                    S.op('dve', [ps, nw0], [y], lambda e, ps=ps, y=y: e.tensor_scalar(y[:, :n], ps[:, :n], -1.0, nw0[:, d, c:c + 1], ALU.mult, ALU.add))
                    ay = tr.get()
                    S.op('act', [y], [ay], lambda e, y=y, ay=ay: e.activation(ay[:, :n], y[:, :n], AF.Abs))
                    S.op('act', [ay], [ay], lambda e, ay=ay: e.activation(ay[:, :n], ay[:, :n], AF.Exp, scale=-1.0))
                    S.op('act', [ay], [ay], lambda e, ay=ay: e.activation(ay[:, :n], ay[:, :n], AF.Ln, bias=1.0, scale=1.0))
                    S.op('dve', [y], [y], lambda e, y=y: e.tensor_scalar_max(y[:, :n], y[:, :n], 0.0))
                    S.op('dve', [y, ay], [y], lambda e, y=y, ay=ay: e.tensor_tensor(y[:, :n], y[:, :n], ay[:, :n], ALU.add))
                    S.op('act', [y], [y], lambda e, y=y: e.activation(y[:, :n], y[:, :n], AF.Exp, bias=-0.5, scale=-1.0))
                    o = orr.get()
                    S.op('act', [y], [o], lambda e, y=y, o=o: e.activation(o[:, :n], y[:, :n], AF.Exp, scale=-1.0))
                    store(4 + 4 * d, c, t0, n, o)
                    ps = psr.get()
                    S.op('pe', [aups, alT], [ps], lambda e, ps=ps: e.matmul(ps[:, :n], aups[ds, cs], alT[ds, ts_], start=True, stop=True))
                    ic = tr.get()
                    S.op('act', [ps, a0s], [ic], lambda e, ps=ps, ic=ic: e.activation(ic[:, :n], ps[:, :n], AF.Sigmoid, bias=a0s[:, d, c:c + 1], scale=1.0))
                    kk = tr.get()
                    S.op('dve', [kS, kks], [kk], lambda e, kk=kk: e.tensor_scalar(kk[:, :n], kS[:, ts_], kks[:, d, c:c + 1], None, ALU.mult))
                    sq = tr.get()
                    S.op('act', [kk], [sq], lambda e, kk=kk, sq=sq: e.activation(sq[:, :n], kk[:, :n], AF.Square))
                    ps = psr.get()
                    S.op('pe', [bones, sq], [ps], lambda e, ps=ps, sq=sq: e.matmul(ps[:, :n], bones[:], sq[:, :n], start=True, stop=True))
                    S.op('act', [ps], [sq], lambda e, ps=ps, sq=sq: e.activation(sq[:, :n], ps[:, :n], AF.Sqrt))
                    S.op('dve', [sq], [sq], lambda e, sq=sq: e.tensor_scalar_max(sq[:, :n], sq[:, :n], 1e-12))
                    S.op('dve', [sq], [sq], lambda e, sq=sq: e.reciprocal(sq[:, :n], sq[:, :n]))
                    S.op('dve', [kk, sq], [kk], lambda e, kk=kk, sq=sq: e.tensor_tensor(kk[:, :n], kk[:, :n], sq[:, :n], ALU.mult))
                    o = orr.get()
                    S.op('dve', [kk], [o], lambda e, kk=kk, o=o: e.tensor_scalar_mul(o[:, :n], kk[:, :n], -1.0))
                    store(6 + 4 * d, c, t0, n, o)
                    o = orr.get()
                    S.op('dve', [kk, ic], [o], lambda e, kk=kk, ic=ic, o=o: e.tensor_tensor(o[:, :n], kk[:, :n], ic[:, :n], ALU.mult))
                    store(7 + 4 * d, c, t0, n, o)
                    S.op('dve', [ic, kas], [ic], lambda e, ic=ic: e.tensor_scalar(ic[:, :n], ic[:, :n], -1.0, kas[:, d, c:c + 1], ALU.add, ALU.mult))
                    S.op('dve', [ic], [ic], lambda e, ic=ic: e.tensor_scalar_add(ic[:, :n], ic[:, :n], 1.0))
                    o = orr.get()
                    S.op('dve', [ic, kS], [o], lambda e, ic=ic, o=o: e.tensor_tensor(o[:, :n], ic[:, :n], kS[:, ts_], ALU.mult))
                    store(5 + 4 * d, c, t0, n, o)
                    S.op('dve', [o, rS, rks], [sq], lambda e, o=o, sq=sq: e.scalar_tensor_tensor(sq[:, :n], o[:, :n], rks[:, c:c + 1], rS[:, ts_], ALU.mult, ALU.mult))
                    S.op('pe', [bones, sq], [pbon], lambda e, sq=sq, d=d: e.matmul(pbon[:, :n], bones[:], sq[:, :n], start=(d == 0), stop=(d == 1)))
                o = orr.get()
                S.op('dve', [pbon, vS], [o], lambda e, o=o: e.tensor_tensor(o[:, :n], pbon[:, :n], vS[:, ts_], ALU.mult))
                store(3, c, t0, n, o)
        S.barrier()


def p4a_inputs(z, j, zT_rk):
    f2 = lambda v: np.ascontiguousarray(v.reshape(2, 8, 128).transpose(2, 0, 1))
    mu = z['rk_mu'][j]
    bones = np.zeros((128, 128), np.float32)
    bones[:64, :64] = 1.0
    bones[64:, 64:] = 1.0
    return {"ident": np.eye(128, dtype=np.float32), "zT": zT_rk, "mu0": fm(mu[0]), "mu1": fm(mu[1]), "g_up": z['rk_g_up'][j],
            "w_up": np.ascontiguousarray(z['rk_w_up'][j].reshape(128, 1024)),
            "a_up": np.ascontiguousarray(z['rk_a_up'][j].reshape(128, 1024)),
            "w0": f2(z['rk_w0'][j]), "a0": f2(z['rk_a0'][j]), "k_k": f2(z['rk_k_k'][j]), "k_a": f2(z['rk_k_a'][j]),
            "r_k": fm(z['rk_r_k'][j].reshape(1024)), "bones": bones}


def emit_p4b(nc, S, io, nsteps=T, G=4, TCH=128):
    X5 = io.inp("X5", [T, 2, 2, 5, 8, 64])
    vT = io.inp("vT", [1024, T])
    Yc = io.out("Yc", [128, 2, 8, T])
    HROW = 5 * 8 * 64
    X5f = X5.rearrange("t j d s q k -> t j d (s q k)")
    vTv = vT.rearrange("(c p) t -> p c t", p=128)
    with contextlib.ExitStack() as st:
        saved_sync = dict(S.self_sync)
        S.self_sync.update({'dve': False, 'pool': False})
        St = sb(nc, st, "St", [128, 2, 8, 64])
        S.op('dve', [], [(St, 0)], lambda e: e.memset(St[:, 0], 0.0))
        S.op('pool', [], [(St, 1)], lambda e: e.memset(St[:, 1], 0.0))
        t1 = sb(nc, st, "t1", [128, 2, 8, 64])
        sa = sb(nc, st, "sa", [128, 2, 8])
        xr = Ring(nc, st, "xb", [128, G, 2, HROW], F32, 2)
        vr = Ring(nc, st, "vc", [128, 2, 8, TCH], F32, 2)
        yr = Ring(nc, st, "yc", [128, 2, 8, TCH], F32, 2)
        engs = ('dve', 'pool')
        vt = yt = None
        xb = None
        lo1 = 0
        for t in range(nsteps):
            tl = t % TCH
            if tl == 0:
                n = min(TCH, nsteps - t)
                assert n == TCH
                vt = vr.get()
                lo1 = int(ORDER_B[t + TCH - 1])
                S.dma(vt[:, 0, :, :], vTv[:, :, t:t + TCH], [], [(vt, 0)])
                S.dma(vt[:, 1, :, :], vTv[:, :, lo1:lo1 + TCH], [], [(vt, 1)])
                yt = yr.get()
            g = t % G
            if g == 0:
                gn = min(G, nsteps - t)
                xb = xr.get()
                tb_hi = int(ORDER_B[t])
                for j in range(2):
                    S.dma(xb[j * 64:(j + 1) * 64, :gn, 0, :], X5f[t:t + gn, j, 0, :].partition_broadcast(64), [], [(xb, 0)])
                    if tb_hi - gn >= 0:
                        srcb = X5f[tb_hi:tb_hi - gn:-1, j, 1, :]
                    else:
                        srcb = X5f[tb_hi::-1, j, 1, :]
                    S.dma(xb[j * 64:(j + 1) * 64, :gn, 1, :], srcb.partition_broadcast(64), [], [(xb, 1)])
            for d in range(2):
                E = engs[d]
                Sd = St[:, d]
                td = t1[:, d]
                xv = xb[:, g, d, :].rearrange("p (s q k) -> p s q k", s=5, q=8)
                W, A, B, Kk, Rr = [xv[:, s_] for s_ in range(5)]
                col = tl if d == 0 else TCH - 1 - tl
                kS, kT, kA = (St, d), (t1, d), (sa, d)
                kX, kV, kY = (xb, d), (vt, d), (yt, d)
                S.op(E, [kS, kX], [kT], lambda e, td=td, Sd=Sd, A=A: e.tensor_tensor(td, Sd, A, ALU.mult))
                S.op('dve', [kT], [kA], lambda e, td=td, d=d: e.reduce_sum(sa[:, d, :], td, AX.X))
                S.op(E, [kS, kX], [kS], lambda e, Sd=Sd, W=W: e.tensor_tensor(Sd, Sd, W, ALU.mult))
                S.op('dve', [kA, kX], [kT], lambda e, td=td, B=B, d=d: e.tensor_tensor(td, B, sa[:, d, :].unsqueeze(2).to_broadcast([128, 8, 64]), ALU.mult))
                S.op(E, [kS, kT], [kS], lambda e, Sd=Sd, td=td: e.tensor_tensor(Sd, Sd, td, ALU.add))
                S.op(E, [kV, kX], [kT], lambda e, td=td, Kk=Kk, d=d, vt=vt, col=col: e.tensor_tensor(td, Kk, vt[:, d, :, col:col + 1].to_broadcast([128, 8, 64]), ALU.mult))
                S.op(E, [kS, kT], [kS], lambda e, Sd=Sd, td=td: e.tensor_tensor(Sd, Sd, td, ALU.add))
                S.op('dve', [kS, kX], [kT], lambda e, td=td, Sd=Sd, Rr=Rr: e.tensor_tensor(td, Sd, Rr, ALU.mult))
                S.op('dve', [kT], [kY], lambda e, td=td, d=d, yt=yt, col=col: e.reduce_sum(yt[:, d, :, col], td, AX.X))
            if tl == TCH - 1:
                c0 = t - tl
                S.dma(Yc[:, 0, :, c0:c0 + TCH], yt[:, 0, :, :], [(yt, 0)], [])
                S.dma(Yc[:, 1, :, lo1:lo1 + TCH], yt[:, 1, :, :], [(yt, 1)], [])
        S.self_sync.update(saved_sync)
        S.barrier()


def p4b_inputs(streams):
    orders = [np.arange(T), ORDER_B]
    X5 = np.empty((T, 2, 2, 5, 8, 64), np.float32)
    Vc = np.empty((128, 2, 8, T), np.float32)
    v = streams[1].reshape(8, 2, 64, T)
    for d in range(2):
        o = orders[d]
        for si, idx in enumerate((4 + 4 * d, 6 + 4 * d, 7 + 4 * d, 5 + 4 * d, 0)):
            s = streams[idx][:, o].reshape(8, 2, 64, T)
            X5[:, :, d, si] = s.transpose(3, 1, 0, 2)
        Vc[:, d] = v[:, :, :, o].transpose(1, 2, 0, 3).reshape(128, 8, T)
    return {"X5": X5.reshape(T, 2, -1), "Vc": Vc}


def p4b_unpack(Yc):
    outs = []
    for d in range(2):
        y = Yc[:, d].reshape(2, 64, 8, T).transpose(2, 0, 1, 3).reshape(1024, T)
        if d == 1:
            y = y[:, INV_B]
        outs.append(np.ascontiguousarray(y))
    return outs


def emit_p4c(nc, S, io):
    di = io.inp
    Yc, bonT, gateT = di("Yc", [128, 2, 8, T]), di("bonT", [1024, T]), di("gateT", [1024, T])
    lnw, lnb = di("lnw", [128, 8]), di("lnb", [128, 8])
    bones_d = di("bones", [128, 128])
    ycT = io.out("ycT", [1024, T])
    with contextlib.ExitStack() as st:
        lw = sb(nc, st, "lw", [128, 8])
        lb = sb(nc, st, "lb", [128, 8])
        bones = sb(nc, st, "bones_s", [128, 128])
        S.dma(lw[:], lnw, [], [lw])
        S.dma(lb[:], lnb, [], [lb])
        S.dma(bones[:], bones_d, [], [bones])
        ir = Ring(nc, st, "in", [128, 512], F32, 8)
        tr = Ring(nc, st, "tr", [128, 512], F32, 6)
        ps1 = Ring(nc, st, "ps1", [128, 512], F32, 2, psum=True)
        ps2 = Ring(nc, st, "ps2", [128, 512], F32, 2, psum=True)
        for c in range(8):
            cs = slice(c * 128, (c + 1) * 128)
            for t0 in range(0, T, 512):
                n = min(512, T - t0)
                a, b, bo, ga = ir.get(), ir.get(), ir.get(), ir.get()
                S.dma(a[:, :n], Yc[:, 0, c, t0:t0 + n], [], [a])
                S.dma(b[:, :n], Yc[:, 1, c, t0:t0 + n], [], [b])
                for tile, src in ((bo, bonT), (ga, gateT)):
                    S.dma(tile[:, :n], src[cs, t0:t0 + n], [], [tile])
                S.op('dve', [a, b], [a], lambda e, a=a, b=b: e.tensor_tensor(a[:, :n], a[:, :n], b[:, :n], ALU.add))
                sq = tr.get()
                S.op('act', [a], [sq], lambda e, a=a, sq=sq: e.activation(sq[:, :n], a[:, :n], AF.Square))
                p1, p2 = ps1.get(), ps2.get()
                S.op('pe', [bones, a], [p1], lambda e, a=a, p1=p1: e.matmul(p1[:, :n], bones[:], a[:, :n], start=True, stop=True))
                S.op('pe', [bones, sq], [p2], lambda e, sq=sq, p2=p2: e.matmul(p2[:, :n], bones[:], sq[:, :n], start=True, stop=True))
                mu = tr.get()
                S.op('act', [p1], [mu], lambda e, p1=p1, mu=mu: e.mul(mu[:, :n], p1[:, :n], 1.0 / 64))
                m2 = tr.get()
                S.op('dve', [mu], [m2], lambda e, mu=mu, m2=m2: e.tensor_tensor(m2[:, :n], mu[:, :n], mu[:, :n], ALU.mult))
                S.op('dve', [p2, m2], [m2], lambda e, p2=p2, m2=m2: e.scalar_tensor_tensor(m2[:, :n], p2[:, :n], 1.0 / 64, m2[:, :n], ALU.mult, ALU.subtract))
                S.op('dve', [m2], [m2], lambda e, m2=m2: e.tensor_scalar_add(m2[:, :n], m2[:, :n], 64e-5))
                S.op('act', [m2], [m2], lambda e, m2=m2: e.activation(m2[:, :n], m2[:, :n], AF.Sqrt))
                S.op('dve', [m2], [m2], lambda e, m2=m2: e.reciprocal(m2[:, :n], m2[:, :n]))
                S.op('dve', [a, mu], [a], lambda e, a=a, mu=mu: e.tensor_tensor(a[:, :n], a[:, :n], mu[:, :n], ALU.subtract))
                S.op('dve', [a, m2], [a], lambda e, a=a, m2=m2: e.tensor_tensor(a[:, :n], a[:, :n], m2[:, :n], ALU.mult))
                S.op('dve', [a, lw, lb], [a], lambda e, a=a: e.tensor_scalar(a[:, :n], a[:, :n], lw[:, c:c + 1], lb[:, c:c + 1], ALU.mult, ALU.add))
                S.op('dve', [a, bo], [a], lambda e, a=a, bo=bo: e.tensor_tensor(a[:, :n], a[:, :n], bo[:, :n], ALU.add))
                S.op('dve', [a, ga], [a], lambda e, a=a, ga=ga: e.tensor_tensor(a[:, :n], a[:, :n], ga[:, :n], ALU.mult))
                S.dma(ycT[cs, t0:t0 + n], a[:, :n], [a], [])
        S.barrier()


def emit_p7(nc, S, io):
    hT = io.inp("hT", [D, T])
    normT = io.inp("normT", [128, KC])
    scT = io.inp("scT", [128, KC, 2])
    shT = io.inp("shT", [128, KC, 2])
    oT = io.out("oT", [D, SEQ])
    with contextlib.ExitStack() as st:
        ones = sb(nc, st, "ones", [128, 128])
        S.op('pool', [], [ones], lambda e: e.memset(ones[:], 1.0))
        geff, shf = emit_geff(S, nc, st, normT, scT, shT, "nf")
        hr = Ring(nc, st, "h", [128, KC, 512], F32, 2)
        sq = Ring(nc, st, "sq", [128, 512], F32, 3)
        tmp = Ring(nc, st, "tmp", [128, 512], F32, 3)
        rstd = sb(nc, st, "rstd", [128, 512])
        ar = Ring(nc, st, "aT", [128, KC, 512], F32, 2)
        ss_ps = st.enter_context(nc.psum_tensor(_PFX[0] + "ss_ps", [128, 512], F32))
        hTv = hT.rearrange("(k p) t -> p k t", p=128)
        oTv = oT.rearrange("(k p) t -> p k t", p=128)
        for (t0, n, col) in TOK_BLOCKS[1:]:
            h_sb = hr.get()
            S.dma(h_sb[:, :, :n], hTv[:, :, t0:t0 + n], [], [h_sb])
            aT = ar.get()
            emit_norm_mod(S, nc, h_sb, n, col, geff, shf, ones, sq, ss_ps, rstd, tmp, aT)
            S.dma(oTv[:, :, t0 - LCTX:t0 - LCTX + n], aT[:, :, :n], [(aT, k) for k in range(KC)], [])
        S.barrier()


class IO:
    def __init__(self, nc, mapping=None):
        self.nc = nc
        self.mapping = mapping

    def _get(self, name, shape, kind):
        if self.mapping is None:
            return self.nc.dram_tensor(name, list(shape), F32, kind=kind).ap()
        ap = self.mapping[name]
        assert list(ap.shape) == list(shape), (name, list(ap.shape), list(shape))
        return ap

    def inp(self, name, shape):
        return self._get(name, shape, "ExternalInput")

    def out(self, name, shape):
        return self._get(name, shape, "ExternalOutput")


def build_standalone(emit_fn, *args, **kw):
    nc = bass.Bass("TRN2", target_bir_lowering=False)
    st = contextlib.ExitStack()
    S = Sched(nc, st)
    emit_fn(nc, S, IO(nc), *args, **kw)
    S.finish()
    st.close()
    return nc


EXT_SPECS = None


def fused_host_inputs(z):
    f32 = lambda a: np.ascontiguousarray(np.asarray(a, dtype=np.float32))
    sh = {}
    for k in ('w_mod', 'b_mod', 'ev_w_in', 'ev_w_out', 'od_w_in', 'od_w_out', 's5_glu_w', 'rk_g_up',
              'moe_w_router', 'moe_w1', 'moe_w2', 'moe_b2'):
        sh[k] = f32(z[k])
    sh['norm1T'] = np.stack([fm(z['norm1'][i]) for i in range(DEPTH)])
    sh['norm2T'] = np.stack([fm(z['norm2'][i]) for i in range(DEPTH)])
    sh['finalT'] = fm(z['final_norm'])
    sh['zeros2'] = np.zeros((128, KC, 2), np.float32)
    sh['ident'] = np.eye(128, dtype=np.float32)
    p2 = [p2_inputs(z, j, np.zeros((1024, T), np.float32)) for j in range(2)]
    for k in ('braw_re', 'braw_im', 'cT_re', 'cT_im', 'lr_row', 'li_row', 'ldt_row', 'lr_p', 'li_p', 'ldt_p'):
        sh['s5_' + k] = np.stack([p2[j][k] for j in range(2)])
    sh['jrow'] = p2[0]['jrow']
    sh['dsk'] = np.stack([fm(z['s5_d'][j]) for j in range(2)])
    sh['glu_b'] = np.stack([fm(z['s5_glu_b'][j]) for j in range(2)])
    sh['na_bias'] = np.stack([na_bias_table(f32(z['na_rpb'][j])) for j in range(2)])
    p4 = [p4a_inputs(z, j, None) for j in range(2)]
    for k in ('mu0', 'mu1', 'w_up', 'a_up', 'w0', 'a0', 'k_k', 'k_a', 'r_k'):
        sh['rk_' + k + '_l'] = np.stack([f32(p4[j][k]) for j in range(2)])
    sh['bones'] = p4[0]['bones']
    sh['lnw'] = np.stack([fm(z['rk_ln_w'][j]) for j in range(2)])
    sh['lnb'] = np.stack([fm(z['rk_ln_b'][j]) for j in range(2)])
    sh['gq'] = np.stack([f32(z['gq_q_norm'][j]).reshape(128, 1) for j in range(2)])
    sh['gk'] = np.stack([f32(z['gq_k_norm'][j]).reshape(128, 1) for j in range(2)])
    C_, Sg_, perm_ = rope_tables()
    sh['ropeC'], sh['ropeS'], sh['perm'] = C_, Sg_, perm_
    p6 = [p6_inputs(z, i, [None] * 6, None, None) for i in range(DEPTH)]
    for k in ('b_r', 'b1g', 'b1l'):
        sh['moe_' + k] = np.stack([f32(p6[i][k]) for i in range(DEPTH)])
    sh['sel'] = p6[0]['sel']
    return sh


def build_fused(shapes, lo=0, hi=DEPTH):
    nc = bass.Bass("TRN2", target_bir_lowering=False)
    ext = {k: nc.dram_tensor(k, list(v), F32, kind="ExternalInput").ap() for k, v in shapes.items()}
    last = (hi == DEPTH)
    oT = nc.dram_tensor("oT", [D, SEQ], F32, kind="ExternalOutput").ap() if last else None
    scr = lambda name, shape: nc.dram_tensor(name, list(shape), F32).ap()
    mod = scr("mod_s", [hi - lo, 2, 6 * D])
    hT = scr("hT_s", [D, T]) if last else nc.dram_tensor("hOut", [D, T], F32, kind="ExternalOutput").ap()
    zT = scr("zT_s", [4992, T])
    v_tm = scr("v_tm_s", [T, 1024])
    yT = scr("yT_s", [2 * 1024, T])
    ysT = scr("ysT_s", [2, 1024, T])
    X5 = scr("X5_s", [T, 2, 2, 5, 8, 64])
    vT = scr("vT_s", [1024, T])
    gateT = scr("gateT_s", [1024, T])
    bonT = scr("bonT_s", [1024, T])
    Yc = scr("Yc_s", [128, 2, 8, T])
    st = contextlib.ExitStack()
    S = Sched(nc, st)

    def modv(li, part):
        return mod[li, :, part * D:(part + 1) * D].rearrange("c (k p) -> p k c", p=128)

    def stage(pfx, fn, mapping, *a, **kw):
        _PFX[0] = pfx
        fn(nc, S, IO(nc, mapping), *a, **kw)
    stage("m_", emit_mod, {"cT": ext["cT"], "w_mod": ext["w_mod"], "b_mod": ext["b_mod"], "mod": mod}, hi - lo)
    for i in range(lo, hi):
        j = 0
        li = i - lo
        even = (i % 2 == 0)
        L = "L%d" % i
        h_in = ext["xT"] if i == lo else hT
        C = 4096 if even else 4992
        vr = (3072, 4096) if even else (RK_IN + 1280, RK_IN + 1536)
        stage(L + "p1_", emit_p1, {"hT": h_in, "normT": ext["norm1T"][li], "scT": modv(li, 1), "shT": modv(li, 0),
                                   "w_in": ext["ev_w_in"][j] if even else ext["od_w_in"][j], "zT": zT[0:C],
                                   "v_tm": v_tm[:, 0:vr[1] - vr[0]]}, C, vr)
        if even:
            m = {"uT": zT[0:1024], "ysT": ysT, "jrow": ext["jrow"]}
            for k in ('braw_re', 'braw_im', 'cT_re', 'cT_im', 'lr_row', 'li_row', 'ldt_row', 'lr_p', 'li_p', 'ldt_p'):
                m[k] = ext['s5_' + k][j]
            stage(L + "p2_", emit_p2, m)
            stage(L + "p2b_", emit_p2b, {"uT": zT[0:1024], "yfT": ysT[0], "ybT": ysT[1], "dsk": ext["dsk"][j],
                                         "glu_w": ext["s5_glu_w"][j], "glu_b": ext["glu_b"][j], "yaT": yT[0:1024]})
            stage(L + "p3_", emit_p3, {"qT": zT[1024:2048], "kT": zT[2048:3072], "v_tm": v_tm, "bias": ext["na_bias"][j],
                                       "ident": ext["ident"], "ybT": yT[1024:2048]})
        else:
            m = {"zT": zT[0:RK_IN], "g_up": ext["rk_g_up"][j], "bones": ext["bones"], "ident": ext["ident"],
                 "vT": vT, "gateT": gateT, "bonT": bonT, "X5": X5}
            for k in ('mu0', 'mu1', 'w_up', 'a_up', 'w0', 'a0', 'k_k', 'k_a', 'r_k'):
                m[k] = ext['rk_' + k + '_l'][j]
            stage(L + "p4a_", emit_p4a, m)
            stage(L + "p4b_", emit_p4b, {"X5": X5, "vT": vT, "Yc": Yc})
            stage(L + "p4c_", emit_p4c, {"Yc": Yc, "bonT": bonT, "gateT": gateT, "lnw": ext["lnw"][j], "lnb": ext["lnb"][j],
                                         "bones": ext["bones"], "ycT": yT[0:1024]})
            stage(L + "p5_", emit_p5, {"qT": zT[RK_IN:RK_IN + 1024], "kT": zT[RK_IN + 1024:RK_IN + 1280], "v_tm": v_tm[:, 0:256],
                                       "gq": ext["gq"][j], "gk": ext["gk"][j], "ropeC": ext["ropeC"], "ropeS": ext["ropeS"],
                                       "perm": ext["perm"], "ident": ext["ident"], "ydT": yT[1024:2048]})
        stage(L + "p6_", emit_p6, {"hT": h_in, "yT": yT, "w_out": ext["ev_w_out"][j] if even else ext["od_w_out"][j],
                                   "g1T": modv(li, 2), "normT": ext["norm2T"][li], "scT": modv(li, 4), "shT": modv(li, 3),
                                   "g2T": modv(li, 5), "w_r": ext["moe_w_router"][li], "b_r": ext["moe_b_r"][li],
                                   "w1": ext["moe_w1"][li], "b1g": ext["moe_b1g"][li], "b1l": ext["moe_b1l"][li],
                                   "w2": ext["moe_w2"][li], "b2": ext["moe_b2"][li], "ident": ext["ident"], "sel": ext["sel"],
                                   "hoT": hT}, NE, i == DEPTH - 1)
    if last:
        stage("f_", emit_p7, {"hT": hT, "normT": ext["finalT"], "scT": ext["zeros2"], "shT": ext["zeros2"], "oT": oT})
    S.finish()
    st.close()
    return nc


_FUSED = {}


DEPTH_KEYS = ('w_mod', 'b_mod', 'norm1T', 'norm2T', 'moe_w_router', 'moe_w1', 'moe_w2', 'moe_b2', 'moe_b_r', 'moe_b1g', 'moe_b1l')
SHARED_KEYS = ('finalT', 'zeros2', 'ident', 'jrow', 'bones', 'ropeC', 'ropeS', 'perm', 'sel')


def kernel(cores=None, **inputs):
    z = inputs
    cores = list(range(NCORES)) if cores is None else list(cores)
    f32 = lambda a: np.ascontiguousarray(np.asarray(a, dtype=np.float32))
    x, c, ctx, c_ctx = f32(z['x']), f32(z['c']), f32(z['ctx']), f32(z['c_ctx'])
    sh = fused_host_inputs(z)
    hcur = [np.ascontiguousarray(np.concatenate([ctx[b], x[b]], 0).T) for b in cores]
    out = None
    for (lo, hi) in ((0, 2), (2, 4)):
        jj = lo // 2
        shl = {}
        for k, v in sh.items():
            if k in DEPTH_KEYS:
                shl[k] = np.ascontiguousarray(v[lo:hi])
            elif k in SHARED_KEYS:
                shl[k] = v
            else:
                shl[k] = np.ascontiguousarray(v[jj:jj + 1])
        in_maps = []
        for bi, b in enumerate(cores):
            m = dict(shl)
            m["xT"] = hcur[bi]
            cc = np.stack([c[b], c_ctx], axis=-1)
            m["cT"] = np.ascontiguousarray(cc.reshape(KC, 128, 2).transpose(1, 0, 2))
            in_maps.append(m)
        key = (lo, hi)
        if key not in _FUSED:
            _FUSED[key] = build_fused({k: v.shape for k, v in in_maps[0].items()}, lo, hi)
        res = run_spmd(_FUSED[key], in_maps)
        if hi == DEPTH:
            out = np.stack([np.ascontiguousarray(r["oT"].T) for r in res], 0).astype(np.float32)
        else:
            hcur = [r["hOut"] for r in res]
    return out
```
